# Optimizing a Trainium2 kernel written in Bass

```python
import jax, jax.numpy as jnp
from jax import lax
import numpy as np

D_MODEL = 2048
BATCH = 4
SEQ = 2048
DEPTH = 2

GRID_W = 64
CTX_LEN = 256
N_MIXERS = 2
EXPAND = 2
INNER = EXPAND * D_MODEL
MLSTM_HEADS = 4
MLSTM_HEAD_DIM = INNER // MLSTM_HEADS
QKV_BLOCK = 4
CONV_K = 3
CHUNK = 128
SGU_GROUPS = 8
N_EXPERTS = 32
TOP_K = 4
D_EXPERT = D_MODEL
SWIGLU_LIMIT = 7.0
SWIGLU_ALPHA = 1.702
MOE_BLOCK = 256
DEEPNORM_ALPHA = (2 * DEPTH) ** 0.25
DEEPNORM_BETA = (8 * DEPTH) ** -0.25
LN_EPS = 1e-5
N_A = (DEPTH + N_MIXERS - 1) // N_MIXERS
N_B = DEPTH // N_MIXERS

kernel_name = 'hybrid_mlstm_chunkmlp_moe_prefix_dit'


def layer_norm(a, g, b):
    af = a.astype(jnp.float32)
    mu = af.mean(-1, keepdims=True)
    var = jnp.mean(jnp.square(af - mu), -1, keepdims=True)
    return ((af - mu) * lax.rsqrt(var + LN_EPS) * g + b).astype(a.dtype)


def modulate(h, shift, scale):
    return h * (1 + scale) + shift


def headwise(a, w):
    B, T, E = a.shape
    return jnp.einsum('btgi,gio->btgo', a.reshape(B, T, E // QKV_BLOCK, QKV_BLOCK), w).reshape(B, T, E)


def grid_conv(a, w, b):
    B, T, E = a.shape
    rows = T // GRID_W
    img = a.reshape(B, rows, GRID_W, E)
    out = lax.conv_general_dilated(img, w[:, :, None, :], (1, 1), 'SAME',
                                   dimension_numbers=('NHWC', 'HWIO', 'NHWC'), feature_group_count=E)
    return out.reshape(B, T, E) + b


def seq_conv(a, w_row, b):
    out = lax.conv_general_dilated(a, w_row[:, None, :], (1,), 'SAME',
                                   dimension_numbers=('NWC', 'WIO', 'NWC'), feature_group_count=a.shape[-1])
    return out + b


def to_heads(a):
    B, T, _ = a.shape
    return a.reshape(B, T, MLSTM_HEADS, MLSTM_HEAD_DIM).transpose(0, 2, 1, 3).astype(jnp.float32)


def mlstm_chunked(q, k, v, ig, lf, state, with_out):
    B, H, T, dh = q.shape
    nc = T // CHUNK

    def to_chunks(a):
        return jnp.moveaxis(a.reshape(a.shape[:2] + (nc, CHUNK) + a.shape[3:]), 2, 0)

    k = k * (dh ** -0.5)
    tril = jnp.tril(jnp.ones((CHUNK, CHUNK), dtype=bool))

    def step(carry, xs):
        C, n, m = carry
        qc, kc, vc, igc, lfc = xs
        b = jnp.cumsum(lfc, axis=-1)
        b_end = b[..., -1]
        w_end = b_end[..., None] - b + igc
        m_new = jnp.maximum(b_end + m, w_end.max(-1))
        decay = jnp.exp(b_end + m - m_new)
        wts = jnp.exp(w_end - m_new[..., None])
        C_new = decay[..., None, None] * C + jnp.einsum('bhs,bhsv,bhsk->bhvk', wts, vc, kc)
        n_new = decay[..., None] * n + jnp.einsum('bhs,bhsk->bhk', wts, kc)
        if not with_out:
            return (C_new, n_new, m_new), None
        log_d = jnp.where(tril, b[..., :, None] - b[..., None, :] + igc[..., None, :], -jnp.inf)
        g = b + m[..., None]
        m_t = jnp.maximum(g, log_d.max(-1))
        dw = jnp.exp(log_d - m_t[..., None])
        inter = jnp.exp(g - m_t)
        s = jnp.einsum('bhtd,bhsd->bhts', qc, kc) * dw
        num = jnp.einsum('bhts,bhsv->bhtv', s, vc) + inter[..., None] * jnp.einsum('bhvk,bhtk->bhtv', C, qc)
        den = s.sum(-1) + inter * jnp.einsum('bhk,bhtk->bht', n, qc)
        h = num / jnp.maximum(jnp.abs(den), jnp.exp(-m_t))[..., None]
        return (C_new, n_new, m_new), h

    xs = (to_chunks(q), to_chunks(k), to_chunks(v), to_chunks(ig), to_chunks(lf))
    state_out, hs = lax.scan(step, state, xs)
    if not with_out:
        return state_out, None
    return state_out, jnp.moveaxis(hs, 0, 2).reshape(B, H, T, dh)


def mlstm_mixer(hx, hc, w_in, conv_w, conv_b, w_q, w_k, w_v, w_gate, b_gate, norm_w, skip, w_out, ctx_out):
    NH, DH = MLSTM_HEADS, MLSTM_HEAD_DIM
    dt = hx.dtype

    def branch(h, conv_fn):
        xm, z = jnp.split(h @ w_in, 2, axis=-1)
        xc = jax.nn.silu(conv_fn(xm))
        q, k, v = headwise(xc, w_q), headwise(xc, w_k), headwise(xm, w_v)
        gates = jnp.concatenate([q, k, v], axis=-1) @ w_gate + b_gate
        return xc, z, to_heads(q), to_heads(k), to_heads(v), jnp.moveaxis(gates.astype(jnp.float32), -1, 1)

    def finish(h, xc, z):
        B, H, T, _ = h.shape
        mu = h.mean(-1, keepdims=True)
        var = jnp.mean(jnp.square(h - mu), -1, keepdims=True)
        hn = ((h - mu) * lax.rsqrt(var + LN_EPS)).transpose(0, 2, 1, 3).reshape(B, T, INNER).astype(dt)
        return ((hn * norm_w + skip * xc) * jax.nn.silu(z)) @ w_out

    xc_x, z_x, qx, kx, vx, gx = branch(hx, lambda a: grid_conv(a, conv_w, conv_b))
    xc_c, z_c, qc, kc, vc, gc = branch(hc, lambda a: seq_conv(a, conv_w[CONV_K // 2], conv_b))
    B = hx.shape[0]
    outs_x, outs_c = [], []
    for d in range(2):
        flip = (lambda a: jnp.flip(a, axis=2)) if d == 1 else (lambda a: a)
        i_sl = slice(2 * d * NH, (2 * d + 1) * NH)
        f_sl = slice((2 * d + 1) * NH, (2 * d + 2) * NH)
        state0 = (jnp.zeros((B, NH, DH, DH), jnp.float32), jnp.zeros((B, NH, DH), jnp.float32),
                  jnp.full((B, NH), -jnp.inf, jnp.float32))
        state_c, h_c = mlstm_chunked(flip(qc), flip(kc), flip(vc), flip(gc[:, i_sl]),
                                     flip(jax.nn.log_sigmoid(gc[:, f_sl])), state0, ctx_out)
        _, h_x = mlstm_chunked(flip(qx), flip(kx), flip(vx), flip(gx[:, i_sl]),
                               flip(jax.nn.log_sigmoid(gx[:, f_sl])), state_c, True)
        outs_x.append(flip(h_x))
        if ctx_out:
            outs_c.append(flip(h_c))
    ox = finish(outs_x[0] + outs_x[1], xc_x, z_x)
    oc = finish(outs_c[0] + outs_c[1], xc_c, z_c) if ctx_out else None
    return ox, oc


def chunk_mlp(h, w_in, ln_g, ln_b, w_s, b_s, w_out):
    B, T, _ = h.shape
    u, v = jnp.split(jax.nn.gelu(h @ w_in, approximate=False), 2, axis=-1)
    v = layer_norm(v, ln_g, ln_b)
    vg = v.reshape(B, T // CHUNK, CHUNK, SGU_GROUPS, INNER // SGU_GROUPS)
    mixed = jnp.einsum('gts,bcsgd->bctgd', w_s, vg) + b_s.T[:, :, None]
    return (u * mixed.reshape(B, T, INNER)) @ w_out


def moe(tok, r_w, r_b, w1, b1, w2, b2):
    T, D = tok.shape
    logits = (tok @ r_w + r_b).astype(jnp.float32)
    top_v, top_i = lax.top_k(logits, TOP_K)
    gate = jax.nn.softmax(top_v, axis=-1)
    flat_e = top_i.reshape(-1)
    n_assign = T * TOP_K
    order = jnp.argsort(flat_e)
    sorted_e = flat_e[order]
    counts = jnp.bincount(flat_e, length=N_EXPERTS)
    starts = jnp.cumsum(counts) - counts
    pcounts = (counts + MOE_BLOCK - 1) // MOE_BLOCK * MOE_BLOCK
    pends = jnp.cumsum(pcounts)
    pstarts = pends - pcounts
    dest_sorted = pstarts[sorted_e] + (jnp.arange(n_assign) - starts[sorted_e])
    n_blocks = -(-n_assign // MOE_BLOCK) + N_EXPERTS
    slot_tok = jnp.zeros((n_blocks * MOE_BLOCK,), jnp.int32).at[dest_sorted].set((order // TOP_K).astype(jnp.int32))
    block_e = jnp.clip(jnp.searchsorted(pends, jnp.arange(n_blocks) * MOE_BLOCK, side='right'), 0, N_EXPERTS - 1)

    def block_fn(args):
        toks, e = args
        hcat = tok[toks] @ w1[e] + b1[e]
        glu = jnp.minimum(hcat[:, :D_EXPERT], SWIGLU_LIMIT)
        lin = jnp.clip(hcat[:, D_EXPERT:], -SWIGLU_LIMIT, SWIGLU_LIMIT)
        act = glu * jax.nn.sigmoid(SWIGLU_ALPHA * glu) * (lin + 1)
        return act @ w2[e] + b2[e]

    y_slots = lax.map(block_fn, (slot_tok.reshape(n_blocks, MOE_BLOCK), block_e))
    dest_flat = jnp.zeros((n_assign,), dest_sorted.dtype).at[order].set(dest_sorted)
    y = y_slots.reshape(n_blocks * MOE_BLOCK, D)[dest_flat].reshape(T, TOP_K, D)
    return jnp.einsum('tk,tkd->td', gate.astype(y.dtype), y)


def setup_inputs(seed: int = 0) -> dict:
    key = jax.random.key(seed)
    ks = iter(jax.random.split(key, 48))

    def nrm(shape, scale=1.0):
        return jax.random.normal(next(ks), shape, jnp.float32) * scale

    D, E, F, NH, NE = D_MODEL, INNER, D_EXPERT, MLSTM_HEADS, N_EXPERTS
    f_bias = jnp.linspace(3.0, 6.0, NH, dtype=jnp.float32)
    return {
        'x': nrm((BATCH, SEQ, D)),
        'c': nrm((BATCH, D)),
        'ctx': nrm((BATCH, CTX_LEN, D)),
        'c_ctx': nrm((D,)),
        'mod_w': nrm((DEPTH, D, 6 * D), 0.5 * D ** -0.5),
        'mod_b': nrm((DEPTH, 6 * D), 0.02),
        'ln1_g': 1.0 + nrm((DEPTH, D), 0.02),
        'ln1_b': nrm((DEPTH, D), 0.02),
        'ln2_g': 1.0 + nrm((DEPTH, D), 0.02),
        'ln2_b': nrm((DEPTH, D), 0.02),
        'a_w_in': nrm((N_A, D, 2 * E), D ** -0.5),
        'a_conv_w': nrm((N_A, CONV_K, CONV_K, E), 1.0 / CONV_K),
        'a_conv_b': nrm((N_A, E), 0.02),
        'a_w_q': nrm((N_A, E // QKV_BLOCK, QKV_BLOCK, QKV_BLOCK), QKV_BLOCK ** -0.5),
        'a_w_k': nrm((N_A, E // QKV_BLOCK, QKV_BLOCK, QKV_BLOCK), QKV_BLOCK ** -0.5),
        'a_w_v': nrm((N_A, E // QKV_BLOCK, QKV_BLOCK, QKV_BLOCK), QKV_BLOCK ** -0.5),
        'a_w_gate': nrm((N_A, 3 * E, 4 * NH), (3 * E) ** -0.5),
        'a_b_gate': jnp.concatenate([nrm((N_A, NH), 0.1), f_bias + nrm((N_A, NH), 0.1),
                                     nrm((N_A, NH), 0.1), f_bias + nrm((N_A, NH), 0.1)], axis=-1),
        'a_norm_w': 1.0 + nrm((N_A, E), 0.02),
        'a_skip': 1.0 + nrm((N_A, E), 0.02),
        'a_w_out': nrm((N_A, E, D), E ** -0.5 * DEEPNORM_BETA),
        'b_w_in': nrm((N_B, D, 2 * E), D ** -0.5),
        'b_ln_g': 1.0 + nrm((N_B, E), 0.02),
        'b_ln_b': nrm((N_B, E), 0.02),
        'b_w_s': nrm((N_B, SGU_GROUPS, CHUNK, CHUNK), CHUNK ** -0.5),
        'b_b_s': 1.0 + nrm((N_B, SGU_GROUPS, CHUNK), 0.02),
        'b_w_out': nrm((N_B, E, D), E ** -0.5 * DEEPNORM_BETA),
        'r_w': nrm((DEPTH, D, NE), D ** -0.5),
        'r_b': nrm((DEPTH, NE), 0.01),
        'e_w1': nrm((DEPTH, NE, D, 2 * F), D ** -0.5),
        'e_b1': nrm((DEPTH, NE, 2 * F), 0.02),
        'e_w2': nrm((DEPTH, NE, F, D), F ** -0.5 * DEEPNORM_BETA),
        'e_b2': nrm((DEPTH, NE, D), 0.02),
    }


def reference(x, c, ctx, c_ctx, mod_w, mod_b, ln1_g, ln1_b, ln2_g, ln2_b,
              a_w_in, a_conv_w, a_conv_b, a_w_q, a_w_k, a_w_v, a_w_gate, a_b_gate, a_norm_w, a_skip, a_w_out,
              b_w_in, b_ln_g, b_ln_b, b_w_s, b_b_s, b_w_out,
              r_w, r_b, e_w1, e_b1, e_w2, e_b2):
    B, S, D = x.shape
    Lc = ctx.shape[1]
    hs, cs = x, ctx
    for layer in range(DEPTH):
        mixer = layer % N_MIXERS
        j = layer // N_MIXERS
        ctx_out = layer < DEPTH - 1
        ctx_in = ctx_out or mixer == 0
        sh1, sc1, g1, sh2, sc2, g2 = [m[:, None, :] for m in
                                      jnp.split(jax.nn.silu(c) @ mod_w[layer] + mod_b[layer], 6, axis=-1)]
        hx = modulate(hs, sh1, sc1)
        hc = None
        if ctx_in:
            csh1, csc1, cg1, csh2, csc2, cg2 = jnp.split(jax.nn.silu(c_ctx) @ mod_w[layer] + mod_b[layer], 6, axis=-1)
            hc = modulate(cs, csh1, csc1)
        if mixer == 0:
            ox, oc = mlstm_mixer(hx, hc, a_w_in[j], a_conv_w[j], a_conv_b[j], a_w_q[j], a_w_k[j], a_w_v[j],
                                 a_w_gate[j], a_b_gate[j], a_norm_w[j], a_skip[j], a_w_out[j], ctx_out)
        else:
            ox = chunk_mlp(hx, b_w_in[j], b_ln_g[j], b_ln_b[j], b_w_s[j], b_b_s[j], b_w_out[j])
            oc = chunk_mlp(hc, b_w_in[j], b_ln_g[j], b_ln_b[j], b_w_s[j], b_b_s[j], b_w_out[j]) if ctx_out else None
        hs = layer_norm(DEEPNORM_ALPHA * hs + g1 * ox, ln1_g[layer], ln1_b[layer])
        if ctx_out:
            cs = layer_norm(DEEPNORM_ALPHA * cs + cg1 * oc, ln1_g[layer], ln1_b[layer])
            tok = jnp.concatenate([modulate(hs, sh2, sc2).reshape(B * S, D),
                                   modulate(cs, csh2, csc2).reshape(B * Lc, D)], axis=0)
        else:
            tok = modulate(hs, sh2, sc2).reshape(B * S, D)
        y = moe(tok, r_w[layer], r_b[layer], e_w1[layer], e_b1[layer], e_w2[layer], e_b2[layer])
        hs = layer_norm(DEEPNORM_ALPHA * hs + g2 * y[:B * S].reshape(B, S, D), ln2_g[layer], ln2_b[layer])
        if ctx_out:
            cs = layer_norm(DEEPNORM_ALPHA * cs + cg2 * y[B * S:].reshape(B, Lc, D), ln2_g[layer], ln2_b[layer])
    return hs
```

```python
from contextlib import ExitStack
import numpy as np
import concourse.bass as bass
import concourse.mybir as mybir
from concourse.bass_utils import run_bass_kernel_spmd

F32 = mybir.dt.float32
BF16 = mybir.dt.bfloat16
AF = mybir.ActivationFunctionType
ALU = mybir.AluOpType
AX = mybir.AxisListType

D = 2048
NT = 2304
NOWN = 1024
E_IN = 4096
NEXP = 32
NCC_DBG = 2
CAP = 384
NSB = CAP // 128
ALPHA = 4.0 ** 0.25
LN_EPS = 1e-5
NEG = -1.0e30
ARENA = 53000

ENG = ['pe', 'act', 'dve', 'pool', 'sp']
NDSEM = 8
_CACHE = {}


class Buf:
    __slots__ = ('ap', 'name', 'w', 'rd')

    def __init__(self, ap, name=''):
        self.ap = ap
        self.name = name
        self.w = None
        self.rd = []

    def __getitem__(self, k):
        return self.ap[k]


class Sched:
    def __init__(self, nc, es):
        self.nc = nc
        self.prog = {e: [] for e in ENG}
        self.esem = {e: es.enter_context(nc.semaphore('s_' + e)) for e in ENG}
        self.ecnt = {e: 0 for e in ENG}
        self.seen = {e: {} for e in ENG}
        self.dq = ['sp', 'act', 'pool']
        self.dsem = {q: [es.enter_context(nc.semaphore('d_%s%d' % (q, i))) for i in range(NDSEM)] for q in self.dq}
        self.dcnt = {q: [0] * NDSEM for q in self.dq}
        self.dn = {q: 0 for q in self.dq}
        self.nins = 0

    def _semof(self, key):
        if key[0] == 'E':
            return self.esem[key[1]]
        return self.dsem[key[1][0]][key[1][1]]

    def _waits(self, eng, deps):
        best = {}
        for t in deps:
            if t is None:
                continue
            key = (t[0], t[1])
            if t[2] > best.get(key, 0):
                best[key] = t[2]
        for key, v in best.items():
            if self.seen[eng].get(key, 0) >= v:
                continue
            self.seen[eng][key] = v
            self.prog[eng].append(('wait', self._semof(key), v))

    def op(self, eng, fn, reads=(), writes=()):
        deps = set()
        for b in reads:
            if b.w is not None and not (eng == 'pe' and b.w[0] == 'E' and b.w[1] == 'pe'):
                deps.add(b.w)
        for b in writes:
            if b.w is not None and not (b.w[0] == 'E' and b.w[1] == eng):
                deps.add(b.w)
            for t in b.rd:
                if not (t[0] == 'E' and t[1] == eng):
                    deps.add(t)
        self._waits(eng, deps)
        self.ecnt[eng] += 1
        tok = ('E', eng, self.ecnt[eng])
        self.prog[eng].append(('ins', fn, self.esem[eng]))
        self.nins += 1
        wset = set(id(b) for b in writes)
        for b in reads:
            if id(b) in wset:
                continue
            b.rd = [t for t in b.rd if not (t[0] == 'E' and t[1] == eng)]
            b.rd.append(tok)
        for b in writes:
            b.w = tok
            b.rd = []
        return tok

    def dma(self, q, out_buf, out_ap, in_buf, in_ap):
        deps = set()
        if in_buf.w is not None:
            deps.add(in_buf.w)
        if out_buf.w is not None:
            deps.add(out_buf.w)
        deps.update(out_buf.rd)
        slot = self.dn[q] % NDSEM
        self.dn[q] += 1
        self.dcnt[q][slot] += 1
        val = 16 * self.dcnt[q][slot]
        if val > 16:
            deps.add(('D', (q, slot), val - 16))
        self._waits(q, deps)
        sem = self.dsem[q][slot]

        def fn(e, out_ap=out_ap, in_ap=in_ap):
            return e.dma_start(out=out_ap, in_=in_ap)
        self.prog[q].append(('dma', fn, sem))
        self.nins += 1
        tok = ('D', (q, slot), val)
        if in_buf.w is not None or True:
            in_buf.rd = [t for t in in_buf.rd if not (t[0] == 'D' and t[1] == (q, slot))]
            in_buf.rd.append(tok)
        out_buf.w = tok
        out_buf.rd = []
        return tok

    def barrier(self):
        toks = set()
        for e in ENG:
            if self.ecnt[e] > 0:
                toks.add(('E', e, self.ecnt[e]))
        for q in self.dq:
            for s in range(NDSEM):
                if self.dcnt[q][s] > 0:
                    toks.add(('D', (q, s), 16 * self.dcnt[q][s]))
        for e in ENG:
            self._waits(e, [t for t in toks if not (t[0] == 'E' and t[1] == e)])

    def emit(self, block):
        prog = self.prog

        def replay(e, items):
            for it in items:
                if it[0] == 'wait':
                    e.wait_ge(it[1], it[2])
                elif it[0] == 'ins':
                    it[1](e).then_inc(it[2], 1)
                else:
                    it[1](e).then_inc(it[2], 16)

        @block.tensor
        def _(e):
            replay(e, prog['pe'])

        @block.scalar
        def _(e):
            replay(e, prog['act'])

        @block.vector
        def _(e):
            replay(e, prog['dve'])

        @block.gpsimd
        def _(e):
            replay(e, prog['pool'])

        @block.sync
        def _(e):
            replay(e, prog['sp'])


C_ID = 0
C_ONES = 128
C_U = 256
C_L = 384
C_TRI = 512
C_BM = 640
C_IOTA = 768
C_PIDX = 1280
NCONST = 1282


def make_consts():
    c = np.zeros((128, NCONST), np.float32)
    p = np.arange(128)
    c[:, C_ID:C_ID + 128] = np.eye(128)
    c[:, C_ONES:C_ONES + 128] = 1.0
    c[:, C_U:C_U + 128] = (p[:, None] <= p[None, :])
    c[:, C_L:C_L + 128] = (p[:, None] >= p[None, :])
    c[:, C_TRI:C_TRI + 128] = (p[:, None] < p[None, :])
    c[:, C_BM:C_BM + 128] = (p[:, None] // 4 == (p[None, :] // 4))
    c[:, C_IOTA:C_IOTA + 512] = np.arange(512)[None, :]
    c[:, C_PIDX] = p
    c[:, C_PIDX + 1] = p + 128
    return c


def colmajor(v, n=None):
    v = np.asarray(v, np.float32).reshape(-1, 128)
    return np.ascontiguousarray(v.T)


IN_SHAPES = {
    'xl': [2048, D], 'ctxl': [256, D], 'cvec': [D, 2], 'consts': [128, NCONST],
    'mod_w': [2, D, 6 * D], 'modb': [128, 192], 'lnp': [128, 2, 4, 16],
    'a_w_in': [D, 2 * E_IN], 'a_cw': [128, 32, 9], 'a_vec': [128, 3, 32],
    'a_w4': [128, 3, 32, 4], 'a_w4t': [128, 3, 32, 4], 'a_wg': [128, 96, 16], 'a_bg': [4, 4],
    'a_w_out': [E_IN, D],
    'b_w_in': [D, 2 * E_IN], 'b_lng': [1, E_IN], 'b_lnb': [1, E_IN], 'b_wsT': [128, 8, 128], 'b_bs': [8, 128],
    'b_w_out': [E_IN, D],
    'r_w': [2, D, NEXP], 'r_b': [2, NEXP], 'e_w1': [2, NEXP, D, 2 * D], 'e_b1': [128, 2, NEXP, 32],
    'e_w2': [2, NEXP, D, D], 'e_b2': [2, NEXP, D],
}


class LazyIn:
    def __init__(self, k):
        self.k = k
        self.d = {}

    def __getitem__(self, name):
        if name not in self.d:
            self.d[name] = self.k.din(name, IN_SHAPES[name])
        return self.d[name]


class KB:
    def __init__(self, nc, es):
        self.nc = nc
        self.es = es
        self.S = Sched(nc, es)
        self.arena = es.enter_context(nc.sbuf_tensor("arena", [128, ARENA], F32))
        self.psum = es.enter_context(nc.psum_tensor("psum", [128, 4096], F32))
        self.pb = [Buf(self.psum[:, i * 512:(i + 1) * 512], 'pb%d' % i) for i in range(8)]
        self.top = 0
        self.hi = ARENA
        self.marks = []
        self.pbi = 0
        self.dbg = {}

    def alloc(self, name, cols, dt=F32):
        n32 = cols if dt == F32 else (cols + 1) // 2
        assert self.top + n32 <= self.hi, (name, self.top, n32, self.hi)
        ap = self.arena[:, self.top:self.top + n32]
        self.top += n32
        if dt != F32:
            ap = ap.bitcast(dt)
        return Buf(ap, name)

    def alloc_hi(self, name, cols):
        self.hi -= cols
        assert self.hi >= self.top, (name, self.top, self.hi)
        return Buf(self.arena[:, self.hi:self.hi + cols], name)

    def mark(self):
        self.marks.append(self.top)

    def release(self):
        self.S.barrier()
        self.top = self.marks.pop()

    def din(self, name, shape, dt=F32):
        return Buf(self.nc.dram_tensor(name, list(shape), dt, kind="ExternalInput").ap(), name)

    def dout(self, name, shape, dt=F32):
        return Buf(self.nc.dram_tensor(name, list(shape), dt, kind="ExternalOutput").ap(), name)

    def dscr(self, name, shape, dt=BF16):
        return Buf(self.nc.dram_tensor(name, list(shape), dt, kind="Internal").ap(), name)

    def bank(self, lo=0, hi=8):
        b = self.pb[lo + self.pbi % (hi - lo)]
        self.pbi += 1
        return b

    def mm(self, ob, oap, lb, lap, rb, rap, start=True, stop=True):
        self.S.op('pe', lambda e: e.matmul(oap, lhsT=lap, rhs=rap, start=start, stop=stop),
                  reads=[lb, rb], writes=[ob])

    def act(self, ob, oap, ib, iap, func, scale=1.0, bias=0.0, extra=()):
        self.S.op('act', lambda e: e.activation(out=oap, in_=iap, func=func, scale=scale, bias=bias),
                  reads=[ib] + list(extra), writes=[ob])

    def ts(self, eng, ob, oap, ib, iap, s1, s2, op0, op1=None, extra=()):
        if op1 is None:
            f = lambda e: e.tensor_scalar(out=oap, in0=iap, scalar1=s1, scalar2=None, op0=op0)
        else:
            f = lambda e: e.tensor_scalar(out=oap, in0=iap, scalar1=s1, scalar2=s2, op0=op0, op1=op1)
        self.S.op(eng, f, reads=[ib] + list(extra), writes=[ob])

    def tt(self, eng, ob, oap, ab, aap, bb, bap, op):
        self.S.op(eng, lambda e: e.tensor_tensor(out=oap, in0=aap, in1=bap, op=op), reads=[ab, bb], writes=[ob])

    def stt(self, eng, ob, oap, ab, aap, sc, bb, bap, op0, op1, extra=()):
        self.S.op(eng, lambda e: e.scalar_tensor_tensor(out=oap, in0=aap, scalar=sc, in1=bap, op0=op0, op1=op1),
                  reads=[ab, bb] + list(extra), writes=[ob])

    def cp(self, eng, ob, oap, ib, iap):
        if eng == 'act':
            self.S.op('act', lambda e: e.copy(out=oap, in_=iap), reads=[ib], writes=[ob])
        else:
            self.S.op(eng, lambda e: e.tensor_copy(out=oap, in_=iap), reads=[ib], writes=[ob])

    def dma(self, q, ob, oap, ib, iap):
        self.S.dma(q, ob, oap, ib, iap)

    def debug_out(self, name, buf, ap, shape, dt=F32):
        o = self.dout(name, shape, dt)
        self.dma('sp', o, o.ap, buf, ap)
        self.dbg[name] = o


def r3(ap, b):
    return ap.rearrange("p (a b) -> p a b", b=b)


def build_program(stop_after=None, dbg=False):
    nc = bass.Bass("TRN2", target_bir_lowering=False)
    es = ExitStack()
    k = KB(nc, es)
    S = k.S
    I = LazyIn(k)
    OUT = k.dout('yl', [NOWN, D])
    k.I = I

    CF = k.alloc('consts', NCONST)
    k.dma('sp', CF, CF.ap, I['consts'], I['consts'].ap)
    CB = k.alloc('constsb', 768, BF16)
    k.cp('dve', CB, CB.ap, CF, CF[:, 0:768])
    k.CF, k.CB = CF, CB
    MODV = k.alloc('modv', 192 * 2)
    MOD1 = k.alloc('mod1', 192 * 2)
    LNP = k.alloc('lnp', 2 * 4 * 16)
    k.dma('sp', LNP, LNP.ap, I['lnp'], I['lnp'].ap.rearrange("p a b c -> p (a b c)"))
    k.MODV, k.MOD1, k.LNP = MODV, MOD1, LNP

    k.EPS = k.alloc('eps', 1)
    k.S.op('dve', lambda e: e.memset(k.EPS.ap, LN_EPS), writes=[k.EPS])
    k.LN32 = k.alloc('ln32', 1)
    k.S.op('dve', lambda e: e.memset(k.LN32.ap, -float(np.log(32.0))), writes=[k.LN32])
    phase_mod(k)
    k.mark()
    k.GTM = k.alloc('gtm', 288)
    k.COLS = k.alloc('cols', 288)
    k.INTB = k.alloc('intb', 144)
    if stop_after == 'mod':
        k.debug_out('d_modv', MODV, MODV.ap, [128, 384])
        return finish(k)
    k.mark()
    phase_hx0(k)
    if stop_after == 'A':
        phase_A(k, ncc=NCC_DBG, dbg_cc=NCC_DBG - 1)
        k.debug_out("d_g", k.Gdbg, k.Gdbg.ap, [128, 1024], BF16)
        k.debug_out('d_gtm', k.GTM, k.GTM.ap, [128, 288])
        k.mark()
        TB = k.alloc('dbgt', 18 * 256, BF16)
        for (nm, scr) in (('d_ktok', k.KTOK), ('d_vtok', k.VTOK)):
            k.dma('sp', TB, r3(TB.ap, 256), scr, scr.ap[:, :, 0:256].rearrange("t p c -> p t c"))
            o = k.dout(nm, [18, 128, 256], BF16)
            k.dma('sp', o, o.ap.rearrange("t p c -> p t c"), TB, r3(TB.ap, 256))
        for (nm, scr) in (('d_qt', k.QT), ('d_kt', k.KT), ('d_szo', k.SZO)):
            k.dma('sp', TB, r3(TB.ap[:, 0:2048], 1024), scr, scr.ap[0:2].rearrange("t p c -> p t c"))
            o = k.dout(nm, [2, 128, NOWN], BF16)
            k.dma('sp', o, o.ap.rearrange("t p c -> p t c"), TB, r3(TB.ap[:, 0:2048], 1024))
        k.release()
        return finish(k)
    if stop_after == 'S':
        phase_A(k, ncc=8)
        k.release()
        phase_G(k)
        k.debug_out('d_cols', k.COLS, k.COLS.ap, [128, 288])
        k.debug_out('d_intb', k.INTB, k.INTB.ap, [128, 144])
        phase_scan(k, heads=(0,), dbg_hs=0)
        k.debug_out('d_gt', k.GT, k.GT[:, 0:8 * NOWN], [128, 8 * NOWN], BF16)
        return finish(k)
    phase_A(k)
    k.release()
    phase_G(k)
    k.mark()
    phase_scan(k)
    k.RS = k.alloc_hi('rs', 16 * NOWN)
    build_rs0(k)
    phase_wout(k, k.GT, 'a_w_out', 0)
    k.release()
    k.release()
    ln_fm(k, 0, 0)
    phase_moe(k, 0)
    ln_fm(k, 0, 2)
    k.mark()
    phase_B(k)
    phase_wout(k, k.G1, 'b_w_out', 1)
    k.release()
    ln_fm(k, 1, 0)
    phase_moe(k, 1)
    ln_fm(k, 1, 2)
    phase_out(k, OUT)
    return finish(k)


def finish(k):
    _CACHE['names'] = list(k.I.d.keys())
    k.S.barrier()
    with k.nc.Block() as block:
        k.S.emit(block)
    k.es.close()
    return k.nc


def mod_col(layer, part, dch):
    return (layer * 6 + part) * 16 + dch


def phase_mod(k):
    I, S = k.I, k.S
    k.mark()
    CV = k.alloc('cv', 32)
    SCV = k.alloc('scv', 32)
    MB = k.alloc('modb', 192)
    k.dma('sp', CV, r3(CV.ap, 2), I['cvec'], I['cvec'].ap.rearrange("(j p) n -> p j n", p=128))
    k.dma('sp', MB, MB.ap, I['modb'], I['modb'].ap)
    k.act(SCV, SCV.ap, CV, CV.ap, AF.Silu)
    scv3 = r3(SCV.ap, 2)
    MW = [k.alloc('mw%d' % i, 16 * 512) for i in range(2)]
    ROW = k.alloc('modrow', 2 * 6 * D)
    n = 0
    for layer in range(2):
        for blk in range(24):
            mw = MW[n % 2]
            n += 1
            src = k.I['mod_w'].ap[layer, :, blk * 512:(blk + 1) * 512].rearrange("(j p) c -> p j c", p=128)
            k.dma('sp' if n % 2 else 'act', mw, r3(mw.ap, 512), I['mod_w'], src)
            mw3 = r3(mw.ap, 512)
            pr = k.bank(1, 8)
            for dch in range(16):
                k.mm(pr, pr[0:2, :], SCV, scv3[:, dch, :], mw, mw3[:, dch, :], start=(dch == 0), stop=(dch == 15))
            c0 = layer * 6 * D + blk * 512
            k.cp('act' if n % 2 else 'dve', ROW, ROW[0:2, c0:c0 + 512], pr, pr[0:2, :])
    ps = k.pb[0]
    ps3 = r3(ps.ap[:, 0:384], 2)
    for j in range(192):
        k.mm(ps, ps3[:, j, :], ROW, ROW[0:2, j * 128:(j + 1) * 128], k.CF, k.CF[0:2, C_ID:C_ID + 2])
    mv3 = r3(k.MODV.ap, 2)
    m13 = r3(k.MOD1.ap, 2)
    for r in range(2):
        k.tt('dve', k.MODV, mv3[:, :, r], ps, ps3[:, :, r], MB, MB.ap, ALU.add)
    k.ts('dve', k.MOD1, k.MOD1.ap, k.MODV, k.MODV.ap, 1.0, None, ALU.add)
    k.release()


def prep_shared(inp):
    f = lambda a: np.ascontiguousarray(np.asarray(a, np.float32))
    sh = {}
    sh['consts'] = make_consts()
    sh['mod_w'] = f(inp['mod_w'])
    sh['modb'] = np.concatenate([colmajor(inp['mod_b'][0]), colmajor(inp['mod_b'][1])], axis=1)
    lnp = np.zeros((128, 2, 4, 16), np.float32)
    for l in range(2):
        for i, nm in enumerate(['ln1_g', 'ln1_b', 'ln2_g', 'ln2_b']):
            lnp[:, l, i, :] = colmajor(inp[nm][l])
    sh['lnp'] = lnp
    sh['a_w_in'] = f(inp['a_w_in'][0])
    sh['a_vec'] = np.stack([colmajor(inp['a_conv_b'][0]), colmajor(inp['a_norm_w'][0]), colmajor(inp['a_skip'][0])], axis=1)
    w4 = np.zeros((128, 3, 32, 4), np.float32)
    w4t = np.zeros((128, 3, 32, 4), np.float32)
    for i, nm in enumerate(['a_w_q', 'a_w_k', 'a_w_v']):
        w = f(inp[nm][0]).reshape(32, 32, 4, 4)
        w4[:, i] = w.transpose(1, 2, 0, 3).reshape(128, 32, 4)
        w4t[:, i] = w.transpose(1, 3, 0, 2).reshape(128, 32, 4)
    sh['a_w4'] = w4
    sh['a_w4t'] = w4t
    sh['a_w_out'] = f(inp['a_w_out'][0])
    sh['b_w_in'] = f(inp['b_w_in'][0])
    sh['b_lng'] = f(inp['b_ln_g'][0]).reshape(1, E_IN)
    sh['b_lnb'] = f(inp['b_ln_b'][0]).reshape(1, E_IN)
    sh['b_w_out'] = f(inp['b_w_out'][0])
    sh['r_w'] = f(inp['r_w'])
    sh['r_b'] = f(inp['r_b'])
    sh['e_w1'] = f(inp['e_w1'])
    sh['e_w2'] = f(inp['e_w2'])
    sh['e_b2'] = f(inp['e_b2'])
    b1 = f(inp['e_b1']).reshape(2, NEXP, 32, 128)
    sh['e_b1'] = np.ascontiguousarray(b1.transpose(3, 0, 1, 2))
    return sh


def prep_core(inp, core, sh):
    f = lambda a: np.ascontiguousarray(np.asarray(a, np.float32))
    b, half = core // 2, core % 2
    d = {}
    x = np.asarray(inp['x'][b], np.float32)
    ctx = np.asarray(inp['ctx'][b], np.float32)
    cw = np.asarray(inp['a_conv_w'][0], np.float32)
    wg = np.asarray(inp['a_w_gate'][0], np.float32)
    bg = np.asarray(inp['a_b_gate'][0], np.float32)
    ws = np.asarray(inp['b_w_s'][0], np.float32)
    bs = np.asarray(inp['b_b_s'][0], np.float32)
    if half == 1:
        x = x[::-1]
        ctx = ctx[::-1]
        cw = cw[::-1, ::-1]
        wg = np.concatenate([wg[:, 8:16], wg[:, 0:8]], axis=1)
        bg = np.concatenate([bg[8:16], bg[0:8]])
        ws = ws[:, ::-1, ::-1]
        bs = bs[:, ::-1]
    d['xl'] = f(x)
    d['ctxl'] = f(ctx)
    d['cvec'] = f(np.stack([np.asarray(inp['c'][b], np.float32), np.asarray(inp['c_ctx'], np.float32)], axis=1))
    d['a_cw'] = f(cw.reshape(9, 32, 128).transpose(2, 1, 0))
    d['a_wg'] = f(wg.reshape(96, 128, 16).transpose(1, 0, 2))
    d['a_bg'] = f(bg.reshape(4, 4).T)
    d['b_wsT'] = f(ws.transpose(2, 0, 1))
    d['b_bs'] = f(bs)
    return d


def phase_hx0(k):
    I = k.I
    CF = k.CF
    HXT = k.alloc('hxt', 16 * NT, BF16)
    k.HXT = HXT
    hx3 = r3(HXT.ap, NT)
    mv3 = r3(k.MODV.ap, 2)
    m13 = r3(k.MOD1.ap, 2)
    k.mark()
    XT = [k.alloc('xt%d' % i, D) for i in range(2)]
    for tt in range(18):
        xt = XT[tt % 2]
        if tt < 2:
            sb, src, r = I['ctxl'], I['ctxl'].ap[tt * 128:(tt + 1) * 128, :], 1
        else:
            sb, src, r = I['xl'], I['xl'].ap[(tt - 2) * 128:(tt - 1) * 128, :], 0
        k.dma('sp', xt, xt.ap, sb, src)
        for g in range(4):
            ps = k.bank()
            for i in range(4):
                dch = g * 4 + i
                k.mm(ps, ps[:, i * 128:(i + 1) * 128], xt, xt[:, dch * 128:(dch + 1) * 128], CF, CF[:, C_ID:C_ID + 128])
            for i in range(4):
                dch = g * 4 + i
                k.act(HXT, hx3[:, dch, tt * 128:(tt + 1) * 128], ps, ps[:, i * 128:(i + 1) * 128], AF.Identity,
                      scale=m13[:, mod_col(0, 1, dch), r:r + 1], bias=mv3[:, mod_col(0, 0, dch), r:r + 1],
                      extra=[k.MOD1, k.MODV])
    k.release()


TBLK = [(0, 512), (512, 512), (1024, 512), (1536, 512), (2048, 256)]


def phase_A(k, ncc=32, dbg_cc=None):
    I, CF, CB = k.I, k.CF, k.CB
    HXT = k.HXT
    hx3 = r3(HXT.ap, NT)
    k.QT = k.dscr('s_qt', [32, 128, NOWN])
    k.KT = k.dscr('s_kt', [32, 128, NOWN])
    k.KTOK = k.dscr('s_ktok', [18, 128, E_IN])
    k.VTOK = k.dscr('s_vtok', [18, 128, E_IN])
    k.XCO = k.dscr('s_xco', [32, 128, NOWN])
    k.SZO = k.dscr('s_szo', [32, 128, NOWN])
    GTM = k.GTM
    k.mark()
    CW = k.alloc('cw', 32 * 9)
    AV = k.alloc('avec', 96)
    k.dma('sp', CW, CW.ap, I['a_cw'], I['a_cw'].ap.rearrange("p a b -> p (a b)"))
    k.dma('sp', AV, AV.ap, I['a_vec'], I['a_vec'].ap.rearrange("p a b -> p (a b)"))
    cw3 = r3(CW.ap, 9)
    av3 = r3(AV.ap, 32)
    BD = k.alloc('bd', 3 * 32 * 128, BF16)
    bd4 = BD.ap.rearrange("p (i c m) -> p i c m", i=3, c=32)
    G = k.alloc('g', 32 * 32, BF16)
    g3 = r3(G.ap, 32)
    k.Gdbg = G
    k.mark()
    W4 = k.alloc('w4', 384)
    W4T = k.alloc('w4t', 384)
    k.dma('sp', W4, W4.ap, I['a_w4'], I['a_w4'].ap.rearrange("p a b c -> p (a b c)"))
    k.dma('sp', W4T, W4T.ap, I['a_w4t'], I['a_w4t'].ap.rearrange("p a b c -> p (a b c)"))
    WGf = k.alloc('wgf', 96 * 16)
    k.dma('sp', WGf, WGf.ap, I['a_wg'], I['a_wg'].ap.rearrange("p a b -> p (a b)"))
    WG = k.alloc('wg', 96 * 16, BF16)
    k.cp('dve', WG, WG.ap, WGf, WGf.ap)
    wg3 = r3(WG.ap, 16)
    BDT = [k.alloc('bdt%d' % i, 3 * 128, BF16) for i in range(2)]
    bm3 = r3(CF[:, C_BM:C_BM + 128], 4)
    w44 = W4.ap.rearrange("p (i c o) -> p i c o", i=3, c=32)
    w4t4 = W4T.ap.rearrange("p (i c o) -> p i c o", i=3, c=32)
    gps = [k.pb[0], k.pb[1]]
    for cc in range(32):
        bdt = BDT[cc % 2]
        bdt3 = r3(bdt.ap, 128)
        for i in range(3):
            eng = 'dve' if (i + cc) % 2 == 0 else 'pool'
            k.tt(eng, BD, r3(bd4[:, i, cc, :], 4), CF, bm3, W4,
                 w44[:, i, cc, :].unsqueeze(1).broadcast_to([128, 32, 4]), ALU.mult)
            k.tt(eng, bdt, r3(bdt3[:, i, :], 4), CF, bm3, W4T,
                 w4t4[:, i, cc, :].unsqueeze(1).broadcast_to([128, 32, 4]), ALU.mult)
        ps = gps[cc // 16]
        c0 = (cc % 16) * 32
        k.mm(ps, ps[:, c0:c0 + 16], bdt, bdt3[:, 0, :], WG, wg3[:, cc, :], start=True, stop=False)
        k.mm(ps, ps[:, c0:c0 + 16], bdt, bdt3[:, 1, :], WG, wg3[:, 32 + cc, :], start=False, stop=True)
        k.mm(ps, ps[:, c0 + 16:c0 + 32], bdt, bdt3[:, 2, :], WG, wg3[:, 64 + cc, :], start=True, stop=True)
    for hb in range(2):
        k.cp('dve', G, G[:, hb * 512:(hb + 1) * 512], gps[hb], gps[hb].ap)
    k.release()

    XM = [k.alloc('xm%d' % i, NT) for i in range(2)]
    ACC = k.alloc('acc', NT)
    XMB = [k.alloc('xmb%d' % i, NT, BF16) for i in range(2)]
    XC = [k.alloc('xc%d' % i, NT, BF16) for i in range(2)]
    QTS = k.alloc('qts', NOWN, BF16)
    KTS = k.alloc('kts', NOWN, BF16)
    SZS = k.alloc('szs', NOWN, BF16)
    KS = k.alloc('ks', 18 * 128, BF16)
    VS = k.alloc('vs', 18 * 128, BF16)
    WI = [k.alloc('wi%d' % i, 16 * 128, BF16) for i in range(3)]
    WZ = [k.alloc('wz%d' % i, 16 * 128, BF16) for i in range(2)]
    k.pbi = 0
    awin = I['a_w_in']

    def load_w(t, col0):
        k.dma('pool', t, r3(t.ap, 128), awin, awin.ap[:, col0:col0 + 128].rearrange("(j p) c -> p j c", p=128))

    load_w(WI[0], 0)
    load_w(WZ[0], E_IN)
    for cc in range(ncc):
        wi, wz = WI[cc % 3], WZ[cc % 2]
        wi3, wz3 = r3(wi.ap, 128), r3(wz.ap, 128)
        if cc + 1 < ncc:
            load_w(WI[(cc + 1) % 3], (cc + 1) * 128)
            load_w(WZ[(cc + 1) % 2], E_IN + (cc + 1) * 128)
        xm, xmb, xc = XM[cc % 2], XMB[cc % 2], XC[cc % 2]
        for (t0, tn) in TBLK:
            ps = k.bank(0, 7)
            for dch in range(16):
                k.mm(ps, ps[:, 0:tn], wi, wi3[:, dch, :], HXT, hx3[:, dch, t0:t0 + tn], start=(dch == 0), stop=(dch == 15))
            k.cp('act', xm, xm[:, t0:t0 + tn], ps, ps[:, 0:tn])
        k.cp('pool', xmb, xmb.ap, xm, xm.ap)
        ce = 'dve'
        k.ts(ce, ACC, ACC.ap, xm, xm.ap, cw3[:, cc, 4:5], None, ALU.mult, extra=[CW])
        xg = r3(xm.ap[:, 256:NT], 64)
        ag = r3(ACC.ap[:, 256:NT], 64)
        for ky in range(3):
            for kx in range(3):
                if ky == 1 and kx == 1:
                    continue
                dy, dx = ky - 1, kx - 1
                y0, y1 = max(0, -dy), 32 - max(0, dy)
                x0, x1 = max(0, -dx), 64 - max(0, dx)
                k.stt(ce, ACC, ag[:, y0:y1, x0:x1], xm, xg[:, y0 + dy:y1 + dy, x0 + dx:x1 + dx],
                      cw3[:, cc, ky * 3 + kx:ky * 3 + kx + 1], ACC, ag[:, y0:y1, x0:x1], ALU.mult, ALU.add, extra=[CW])
        for kx in (0, 2):
            dx = kx - 1
            x0, x1 = max(0, -dx), 256 - max(0, dx)
            k.stt(ce, ACC, ACC[:, x0:x1], xm, xm[:, x0 + dx:x1 + dx], cw3[:, cc, 3 + kx:3 + kx + 1],
                  ACC, ACC[:, x0:x1], ALU.mult, ALU.add, extra=[CW])
        k.act(xc, xc.ap, ACC, ACC.ap, AF.Silu, bias=av3[:, 0, cc:cc + 1], extra=[AV])
        if dbg_cc == cc:
            k.debug_out('d_xc', xc, xc.ap, [128, NT], BF16)
            k.debug_out('d_xm', xm, xm.ap, [128, NT])
        for (i, dst, scr) in ((0, QTS, k.QT), (1, KTS, k.KT)):
            for hb in range(2):
                ps = k.bank(0, 7)
                k.mm(ps, ps.ap, BD, bd4[:, i, cc, :], xc, xc[:, 256 + hb * 512:256 + (hb + 1) * 512])
                k.cp('act', dst, dst[:, hb * 512:(hb + 1) * 512], ps, ps.ap)
            k.dma('sp', scr, scr.ap[cc], dst, dst.ap)
        for (src, i, dst, scr) in ((xc, 1, KS, k.KTOK), (xmb, 2, VS, k.VTOK)):
            for g0 in range(0, 18, 4):
                ps = k.bank(0, 7)
                n = min(4, 18 - g0)
                for j in range(n):
                    tt = g0 + j
                    k.mm(ps, ps[:, j * 128:(j + 1) * 128], src, src[:, tt * 128:(tt + 1) * 128], BD, bd4[:, i, cc, :])
                k.cp('act', dst, dst[:, g0 * 128:(g0 + n) * 128], ps, ps[:, 0:n * 128])
            k.dma('sp', scr, scr.ap[:, :, cc * 128:(cc + 1) * 128].rearrange("t p c -> p t c"), dst, r3(dst.ap, 128))
        GPS = k.bank(0, 7)
        gps3 = r3(GPS.ap[:, 0:288], 16)
        for tt in range(18):
            k.mm(GPS, gps3[:, tt, :], xc, xc[:, tt * 128:(tt + 1) * 128], G, g3[:, cc, 0:16], start=True, stop=False)
            k.mm(GPS, gps3[:, tt, :], xmb, xmb[:, tt * 128:(tt + 1) * 128], G, g3[:, cc, 16:32], start=False, stop=True)
        if cc == 0:
            k.cp('dve', GTM, GTM.ap, GPS, GPS[:, 0:288])
        else:
            k.tt('dve', GTM, GTM.ap, GPS, GPS[:, 0:288], GTM, GTM.ap, ALU.add)
        k.dma('sp', k.XCO, k.XCO.ap[cc], xc, xc[:, 256:256 + NOWN])
        for hb in range(2):
            ps = k.bank(0, 7)
            for dch in range(16):
                k.mm(ps, ps.ap, wz, wz3[:, dch, :], HXT, hx3[:, dch, 256 + hb * 512:256 + (hb + 1) * 512],
                     start=(dch == 0), stop=(dch == 15))
            k.act(SZS, SZS[:, hb * 512:(hb + 1) * 512], ps, ps.ap, AF.Silu)
        k.dma('sp', k.SZO, k.SZO.ap[cc], SZS, SZS.ap)
    k.release()


def seq_order(d):
    if d == 0:
        return [(0, False), (1, False)] + [(2 + c, True) for c in range(8)]
    return [(1, False), (0, False)] + [(2 + c, False) for c in range(15, 7, -1)] + [(2 + c, True) for c in range(7, -1, -1)]


def phase_G(k):
    I, CF = k.I, k.CF
    COLS, INTB = k.COLS, k.INTB
    SC = k.dscr('s_inter', [2, 4, 18], F32)
    k.mark()
    GR = k.alloc('gr', NT)
    GK = k.alloc('gk', 4 * NT)
    RW = k.alloc('rw', NT)
    BG = k.alloc('bg', 4)
    k.dma('sp', BG, BG[0:4, :], I['a_bg'], I['a_bg'].ap)
    gtm3 = r3(k.GTM.ap, 16)
    for tt in range(18):
        pbk = k.pb[(tt * 128) // 512]
        c0 = (tt * 128) % 512
        k.mm(pbk, pbk[0:16, c0:c0 + 128], k.GTM, gtm3[:, tt, :], CF, CF[:, C_ID:C_ID + 128])
    for bk in range(5):
        n = 512 if bk < 4 else 256
        k.cp('act', GR, GR[0:16, bk * 512:bk * 512 + n], k.pb[bk], k.pb[bk][0:16, 0:n])
    for kind in range(4):
        k.dma('sp', GK, GK[0:4, kind * NT:(kind + 1) * NT], GR, GR[kind * 4:(kind + 1) * 4, :])
    T = [k.alloc('gt%d' % i, NT) for i in range(8)]
    SM = k.alloc('gsm', 128)
    R4 = lambda b: b[0:4, :]
    V3 = lambda b: r3(b[0:4, :], 128)
    for d in range(2):
        IG, Z, AZ, LF, B0, B1, A, U = T
        k.ts('dve', IG, R4(IG), GK, GK[0:4, (2 * d) * NT:(2 * d + 1) * NT], BG[0:4, 2 * d:2 * d + 1], None, ALU.add, extra=[BG])
        k.ts('dve', Z, R4(Z), GK, GK[0:4, (2 * d + 1) * NT:(2 * d + 2) * NT], BG[0:4, 2 * d + 1:2 * d + 2], None, ALU.add, extra=[BG])
        k.act(AZ, R4(AZ), Z, R4(Z), AF.Abs)
        k.act(AZ, R4(AZ), AZ, R4(AZ), AF.Exp, scale=-1.0)
        k.act(AZ, R4(AZ), AZ, R4(AZ), AF.Ln, bias=1.0)
        k.ts('dve', LF, R4(LF), Z, R4(Z), 0.0, None, ALU.min)
        k.tt('dve', B0, R4(B0), LF, R4(LF), AZ, R4(AZ), ALU.subtract)
        cur, nxt = B0, B1
        sh = 1
        while sh < 128:
            c3, n3 = V3(cur), V3(nxt)
            if d == 0:
                k.cp('dve', nxt, n3[:, :, 0:sh], cur, c3[:, :, 0:sh])
                k.tt('dve', nxt, n3[:, :, sh:128], cur, c3[:, :, sh:128], cur, c3[:, :, 0:128 - sh], ALU.add)
            else:
                k.cp('dve', nxt, n3[:, :, 128 - sh:128], cur, c3[:, :, 128 - sh:128])
                k.tt('dve', nxt, n3[:, :, 0:128 - sh], cur, c3[:, :, 0:128 - sh], cur, c3[:, :, sh:128], ALU.add)
            cur, nxt = nxt, cur
            sh *= 2
        Bc = cur
        Tm = nxt
        k.tt('dve', A, R4(A), IG, R4(IG), Bc, R4(Bc), ALU.subtract)
        AMAX, MC, MM, INTER = SM[0:4, 0:18], SM[0:4, 18:36], SM[0:4, 36:54], SM[0:4, 54:72]
        k.S.op('dve', lambda e, A=A, AMAX=AMAX: e.tensor_reduce(out=AMAX, in_=V3(A), axis=AX.X, op=ALU.max), reads=[A], writes=[SM])
        bend = V3(Bc)[:, :, 127] if d == 0 else V3(Bc)[:, :, 0]
        order = [t for (t, _) in seq_order(d)] if d == 1 else list(range(18))
        k.S.op('dve', lambda e, MM=MM: e.memset(MM, NEG), writes=[SM])
        for i, t in enumerate(order):
            k.tt('dve', SM, MC[:, t:t + 1], SM, MM[:, t:t + 1], SM, AMAX[:, t:t + 1], ALU.max)
            if i + 1 < len(order):
                tn = order[i + 1]
                k.tt('dve', SM, MM[:, tn:tn + 1], Bc, bend[:, t:t + 1], SM, MC[:, t:t + 1], ALU.add)
        k.tt('dve', SM, INTER, SM, MM, SM, MC, ALU.subtract)
        k.act(SM, INTER, SM, INTER, AF.Exp)
        k.dma('sp', SC, SC.ap[d], SM, INTER)
        mcb = MC.unsqueeze(2).broadcast_to([4, 18, 128])
        k.tt('dve', U, V3(U), A, V3(A), SM, mcb, ALU.subtract)
        k.act(U, R4(U), U, R4(U), AF.Exp, bias=k.LN32[0:4, 0:1], extra=[k.LN32])
        k.tt('dve', Tm, V3(Tm), Bc, V3(Bc), SM, mcb, ALU.add)
        k.act(Tm, R4(Tm), Tm, R4(Tm), AF.Exp, scale=-1.0)
        k.dma('sp', RW, RW[(2 * d) * 4:(2 * d) * 4 + 4, :], U, R4(U))
        k.dma('sp', RW, RW[(2 * d + 1) * 4:(2 * d + 1) * 4 + 4, :], Tm, R4(Tm))
    ps = k.pb[5]
    for tt in range(18):
        k.mm(ps, ps[:, tt * 16:(tt + 1) * 16], RW, RW[0:16, tt * 128:(tt + 1) * 128], CF, CF[0:16, C_ID:C_ID + 16])
    k.cp('dve', COLS, COLS.ap, ps, ps[:, 0:288])
    k.dma('sp', INTB, INTB.ap, SC, SC.ap.rearrange("d h t -> (d h t)").partition_broadcast(128))
    k.release()


def phase_scan(k, heads=(0, 1, 2, 3), dbg_hs=None):
    I, CF, CB = k.I, k.CF, k.CB
    COLS, INTB = k.COLS, k.INTB
    cols3 = r3(COLS.ap, 16)
    GT = k.alloc('gt', 32 * NOWN, BF16)
    k.GT = GT
    gt3 = r3(GT.ap, NOWN)
    k.mark()
    AV = k.alloc('avec', 96)
    k.dma('sp', AV, AV.ap, I['a_vec'], I['a_vec'].ap.rearrange("p a b -> p (a b)"))
    av3 = r3(AV.ap, 32)
    CT = [k.alloc('ct%d' % j, 1024) for j in range(8)]
    CTB = [k.alloc('ctb%d' % j, 1024, BF16) for j in range(8)]
    NV = k.alloc('nv', 8)
    NVB = k.alloc('nvb', 8, BF16)
    HS = [k.alloc('hs%d' % c, 1024) for c in range(8)]
    KK = [k.alloc('kk%d' % i, 1024, BF16) for i in range(2)]
    VV = [k.alloc('vv%d' % i, 1024, BF16) for i in range(2)]
    QC = [k.alloc('qc%d' % i, 1024, BF16) for i in range(2)]
    KC = [k.alloc('kc%d' % i, 1024, BF16) for i in range(2)]
    KP = k.alloc('kp', 1024, BF16)
    QTI = k.alloc('qti', 1024, BF16)
    ST = k.alloc('st', 128, BF16)
    DN = k.alloc('dn', 2)
    HN = k.alloc('hn', 8 * 1024, BF16)
    hn3 = r3(HN.ap, 1024)
    STAT = k.alloc('stat', 2 * 6 + 2 + 2)
    SX, TMPF = HS[0], HS[1]
    XCL = [HS[2], HS[3]]
    SZL = [HS[4], HS[5]]
    ONEB = CB[:, C_ONES:C_ONES + 1]
    IDB = CB[:, C_ID:C_ID + 128]
    P_S, P_D = k.pb[0], k.pb[1]
    P_N = Buf(k.psum[:, 1024:2048], 'pn')
    P_C = [Buf(k.psum[:, 2048:3072], 'pc0'), Buf(k.psum[:, 3072:4096], 'pc1')]
    npc = 0
    for h in heads:
        for d in range(2):
            for j in range(8):
                k.S.op('pool', lambda e, t=CT[j]: e.memset(t.ap, 0.0), writes=[CT[j]])
                k.S.op('pool', lambda e, t=CTB[j]: e.memset(t.ap, 0.0), writes=[CTB[j]])
            k.S.op('pool', lambda e: e.memset(NV.ap, 0.0), writes=[NV])
            k.S.op('pool', lambda e: e.memset(NVB.ap, 0.0), writes=[NVB])
            seq = seq_order(d)
            maskT = CB[:, C_U:C_U + 128] if d == 0 else CB[:, C_L:C_L + 128]

            def loads(i):
                tile, wo = seq[i]
                kk, vv = KK[i % 2], VV[i % 2]
                k.dma('sp', kk, kk.ap, k.KTOK, k.KTOK.ap[tile, :, h * 1024:(h + 1) * 1024])
                k.dma('act', vv, vv.ap, k.VTOK, k.VTOK.ap[tile, :, h * 1024:(h + 1) * 1024])
                if wo:
                    c = tile - 2
                    qc, kc = QC[i % 2], KC[i % 2]
                    k.dma('sp', qc, r3(qc.ap, 128), k.QT, k.QT.ap[h * 8:(h + 1) * 8, :, c * 128:(c + 1) * 128].rearrange("j p t -> p j t"))
                    k.dma('act', kc, r3(kc.ap, 128), k.KT, k.KT.ap[h * 8:(h + 1) * 8, :, c * 128:(c + 1) * 128].rearrange("j p t -> p j t"))
            loads(0)
            for i, (tile, wo) in enumerate(seq):
                if i + 1 < len(seq):
                    loads(i + 1)
                kk, vv, qc, kc = KK[i % 2], VV[i % 2], QC[i % 2], KC[i % 2]
                qc3, kc3 = r3(qc.ap, 128), r3(kc.ap, 128)
                ucol = cols3[:, tile, (d * 2) * 4 + h:(d * 2) * 4 + h + 1]
                fcol = cols3[:, tile, (d * 2 + 1) * 4 + h:(d * 2 + 1) * 4 + h + 1]
                icol = INTB[:, d * 72 + h * 18 + tile:d * 72 + h * 18 + tile + 1]
                k.act(KP, KP.ap, kk, kk.ap, AF.Copy, scale=ucol, extra=[COLS])
                if wo:
                    c = tile - 2
                    for j in range(8):
                        k.mm(P_S, P_S[:, 0:128], kc, kc3[:, j, :], qc, qc3[:, j, :], start=(j == 0), stop=(j == 7))
                    k.stt('dve', ST, ST.ap, P_S, P_S[:, 0:128], ucol, CB, maskT, ALU.mult, ALU.mult, extra=[COLS])
                    k.ts('dve', QTI, QTI.ap, qc, qc.ap, icol, None, ALU.mult, extra=[INTB])
                    qti3 = r3(QTI.ap, 128)
                    for half in range(2):
                        k.mm(P_N, P_N[:, half * 512:(half + 1) * 512], ST, ST.ap, vv, vv[:, half * 512:(half + 1) * 512], start=True, stop=False)
                        for j in range(8):
                            k.mm(P_N, P_N[:, half * 512:(half + 1) * 512], QTI, qti3[:, j, :], CTB[j], CTB[j][:, half * 512:(half + 1) * 512],
                                 start=False, stop=(j == 7))
                    k.mm(P_D, P_D[:, 0:1], ST, ST.ap, CB, ONEB, start=True, stop=False)
                    for j in range(8):
                        k.mm(P_D, P_D[:, 0:1], QTI, qti3[:, j, :], NVB, NVB[:, j:j + 1], start=False, stop=(j == 7))
                    k.act(DN, DN[:, 0:1], P_D, P_D[:, 0:1], AF.Abs)
                    k.tt('dve', DN, DN[:, 0:1], DN, DN[:, 0:1], COLS, fcol, ALU.max)
                    k.S.op('dve', lambda e: e.reciprocal(out=DN[:, 1:2], in_=DN[:, 0:1]), reads=[DN], writes=[DN])
                    if d == 0:
                        k.act(HS[c], HS[c].ap, P_N, P_N.ap, AF.Copy, scale=DN[:, 1:2], extra=[DN])
                    else:
                        k.stt('dve', HS[c], HS[c].ap, P_N, P_N.ap, DN[:, 1:2], HS[c], HS[c].ap, ALU.mult, ALU.add, extra=[DN])
                for j in range(8):
                    pc = P_C[npc % 2]
                    npc += 1
                    for half in range(2):
                        k.mm(pc, pc[:, half * 512:(half + 1) * 512], KP, KP[:, j * 128:(j + 1) * 128], vv, vv[:, half * 512:(half + 1) * 512])
                    k.stt('dve', CT[j], CT[j].ap, CT[j], CT[j].ap, icol, pc, pc.ap, ALU.mult, ALU.add, extra=[INTB])
                    k.cp('act', CTB[j], CTB[j].ap, CT[j], CT[j].ap)
                for j in range(8):
                    k.mm(P_D, P_D[:, 8 + j:9 + j], KP, KP[:, j * 128:(j + 1) * 128], CB, ONEB)
                k.stt('dve', NV, NV.ap, NV, NV.ap, icol, P_D, P_D[:, 8:16], ALU.mult, ALU.add, extra=[INTB])
                k.cp('dve', NVB, NVB.ap, NV, NV.ap)
        if dbg_hs == h:
            for c in range(8):
                k.debug_out('d_hs%d' % c, HS[c], HS[c].ap, [128, 1024])
        for c in range(8):
            st6 = r3(STAT[:, 0:12], 6)
            for q in range(2):
                k.S.op('dve', lambda e, c=c, q=q: e.bn_stats(out=st6[:, q, :], in_=HS[c][:, q * 512:(q + 1) * 512]), reads=[HS[c]], writes=[STAT])
            k.S.op('dve', lambda e: e.bn_aggr(out=STAT[:, 12:14], in_=st6), reads=[STAT], writes=[STAT])
            k.act(STAT, STAT[:, 15:16], STAT, STAT[:, 13:14], AF.Sqrt, bias=k.EPS[:, 0:1], extra=[k.EPS])
            k.S.op('dve', lambda e: e.reciprocal(out=STAT[:, 14:15], in_=STAT[:, 15:16]), reads=[STAT], writes=[STAT])
            k.ts('dve', HN, hn3[:, c, :], HS[c], HS[c].ap, STAT[:, 12:13], STAT[:, 14:15], ALU.subtract, ALU.mult, extra=[STAT])
        for j in range(8):
            cc = h * 8 + j
            xcl, szl = XCL[j % 2], SZL[j % 2]
            xcl_ap = xcl.ap[:, 0:512].bitcast(BF16)
            szl_ap = szl.ap[:, 0:512].bitcast(BF16)
            k.dma('sp', xcl, xcl_ap, k.XCO, k.XCO.ap[cc])
            k.dma('act', szl, szl_ap, k.SZO, k.SZO.ap[cc])
            for c in range(8):
                k.mm(P_N, P_N[:, c * 128:(c + 1) * 128], HN, hn3[:, c, j * 128:(j + 1) * 128], CB, IDB)
            k.act(SX, SX.ap, xcl, xcl_ap, AF.Copy, scale=av3[:, 2, cc:cc + 1], extra=[AV])
            k.stt('dve', TMPF, TMPF.ap, P_N, P_N.ap, av3[:, 1, cc:cc + 1], SX, SX.ap, ALU.mult, ALU.add, extra=[AV])
            k.tt('pool', GT, gt3[:, cc, :], TMPF, TMPF.ap, szl, szl_ap, ALU.mult)
    k.release()


def lnp_col(k, layer, which, dch):
    v = k.LNP.ap.rearrange("p (a b c) -> p a b c", a=2, b=4)
    return v[:, layer, which, dch:dch + 1]


def modv(k, layer, part, dch, r=0):
    return r3(k.MODV.ap, 2)[:, mod_col(layer, part, dch), r:r + 1]


def mod1(k, layer, part, dch, r=0):
    return r3(k.MOD1.ap, 2)[:, mod_col(layer, part, dch), r:r + 1]


def build_rs0(k):
    I, CF = k.I, k.CF
    rs3 = r3(k.RS.ap, NOWN)
    k.mark()
    XT = [k.alloc('xt%d' % i, D) for i in range(2)]
    for tt in range(8):
        xt = XT[tt % 2]
        k.dma('sp', xt, xt.ap, I['xl'], I['xl'].ap[tt * 128:(tt + 1) * 128, :])
        for g in range(4):
            ps = k.bank()
            for i in range(4):
                dch = g * 4 + i
                k.mm(ps, ps[:, i * 128:(i + 1) * 128], xt, xt[:, dch * 128:(dch + 1) * 128], CF, CF[:, C_ID:C_ID + 128])
            k.act(k.RS, rs3[:, g * 4:(g + 1) * 4, tt * 128:(tt + 1) * 128], ps, r3(ps.ap, 128), AF.Copy, scale=ALPHA)
    k.release()


def phase_wout(k, GT, wname, layer):
    I = k.I
    gt3 = r3(GT.ap, NOWN)
    rs3 = r3(k.RS.ap, NOWN)
    k.mark()
    WO = [k.alloc('wo%d' % i, 32 * 256, BF16) for i in range(2)]
    w = I[wname]
    for p in range(8):
        wo = WO[p % 2]
        wo3 = r3(wo.ap, 256)
        k.dma('pool', wo, wo3, w, w.ap[:, p * 256:(p + 1) * 256].rearrange("(c p) d -> p c d", p=128))
        for dl in range(2):
            dch = p * 2 + dl
            for half in range(2):
                ps = k.bank()
                for cc in range(32):
                    k.mm(ps, ps.ap, wo, wo3[:, cc, dl * 128:(dl + 1) * 128], GT, gt3[:, cc, half * 512:(half + 1) * 512],
                         start=(cc == 0), stop=(cc == 31))
                k.stt('dve', k.RS, rs3[:, dch, half * 512:(half + 1) * 512], ps, ps.ap, modv(k, layer, 2, dch),
                      k.RS, rs3[:, dch, half * 512:(half + 1) * 512], ALU.mult, ALU.add, extra=[k.MODV])
    k.release()


def ln_fm(k, layer, which):
    CF = k.CF
    rs3 = r3(k.RS.ap, NOWN)
    k.mark()
    SQ = [k.alloc('sq%d' % i, 512) for i in range(2)]
    MEAN = k.alloc('mean', 512)
    RSTD = k.alloc('rstd', 512)
    M2 = k.alloc('m2', 512)
    T = [k.alloc('lt%d' % i, 512) for i in range(2)]
    ONES = CF[:, C_ONES:C_ONES + 128]
    for half in range(2):
        sl = slice(half * 512, (half + 1) * 512)
        ps_s, ps_q = k.bank(), k.bank()
        for dch in range(16):
            k.mm(ps_s, ps_s.ap, CF, ONES, k.RS, rs3[:, dch, sl], start=(dch == 0), stop=(dch == 15))
        for dch in range(16):
            sq = SQ[dch % 2]
            k.act(sq, sq.ap, k.RS, rs3[:, dch, sl], AF.Square)
            k.mm(ps_q, ps_q.ap, CF, ONES, sq, sq.ap, start=(dch == 0), stop=(dch == 15))
        k.act(MEAN, MEAN.ap, ps_s, ps_s.ap, AF.Copy, scale=1.0 / D)
        k.tt('pool', M2, M2.ap, MEAN, MEAN.ap, MEAN, MEAN.ap, ALU.mult)
        k.stt('dve', RSTD, RSTD.ap, ps_q, ps_q.ap, 1.0 / D, M2, M2.ap, ALU.mult, ALU.subtract)
        k.act(RSTD, RSTD.ap, RSTD, RSTD.ap, AF.Sqrt, bias=k.EPS[:, 0:1], extra=[k.EPS])
        k.S.op('dve', lambda e: e.reciprocal(out=RSTD.ap, in_=RSTD.ap), reads=[RSTD], writes=[RSTD])
        for dch in range(16):
            t = T[dch % 2]
            k.tt('dve', t, t.ap, k.RS, rs3[:, dch, sl], MEAN, MEAN.ap, ALU.subtract)
            k.tt('pool', t, t.ap, t, t.ap, RSTD, RSTD.ap, ALU.mult)
            k.act(k.RS, rs3[:, dch, sl], t, t.ap, AF.Identity, scale=lnp_col(k, layer, which, dch),
                  bias=lnp_col(k, layer, which + 1, dch), extra=[k.LNP])
    k.release()


def phase_moe(k, layer, nexp=NEXP):
    I, CF, CB = k.I, k.CF, k.CB
    rs3 = r3(k.RS.ap, NOWN)
    IDB = CB[:, C_ID:C_ID + 128]
    k.mark()
    TOK = k.alloc('tok', 8 * D, BF16)
    tok3 = r3(TOK.ap, D)
    MASK = k.alloc('mask', 256)
    RANK = k.alloc('rank', 256)
    GHL = k.alloc('ghl', 512, BF16)
    MASKB = k.alloc('maskb', 256, BF16)
    mask3, rank3, maskb3 = [r3(b.ap, 32) for b in (MASK, RANK, MASKB)]
    ghl4 = GHL.ap.rearrange("p (t e two) -> p t e two", t=8, two=2)
    B1E = [k.alloc('b1e%d' % i, 32) for i in range(2)]
    k.mark()
    LG = k.alloc('lg', 256)
    GATE = k.alloc('gate', 256)
    lg3, gate3 = r3(LG.ap, 32), r3(GATE.ap, 32)
    TKF = k.alloc('tkf', 16 * NOWN)
    tkf3 = r3(TKF.ap, NOWN)
    TKB = [k.alloc('tkb%d' % i, NOWN, BF16) for i in range(2)]
    RWF = k.alloc('rwf', 16 * 32)
    RB = k.alloc('rb', 32)
    SMT = k.alloc('smt', 64)
    k.dma('sp', RWF, r3(RWF.ap, 32), I['r_w'], I['r_w'].ap[layer].rearrange("(j p) e -> p j e", p=128))
    k.dma('sp', RB, RB.ap, I['r_b'], I['r_b'].ap[layer, :].partition_broadcast(128))
    rwf3 = r3(RWF.ap, 32)
    for dch in range(16):
        k.act(TKF, tkf3[:, dch, :], k.RS, rs3[:, dch, :], AF.Identity, scale=mod1(k, layer, 4, dch), bias=modv(k, layer, 3, dch),
              extra=[k.MOD1, k.MODV])
        tkb = TKB[dch % 2]
        k.cp('pool', tkb, tkb.ap, TKF, tkf3[:, dch, :])
        for tg in range(2):
            ps = k.bank()
            for j in range(4):
                t = tg * 4 + j
                k.mm(ps, ps[:, j * 128:(j + 1) * 128], tkb, tkb[:, t * 128:(t + 1) * 128], CB, IDB)
            k.cp('dve', TOK, tok3[:, tg * 4:(tg + 1) * 4, dch * 128:(dch + 1) * 128], ps, r3(ps.ap, 128))
    for dch in range(16):
        k.S.op('pool', lambda e, dch=dch: e.tensor_scalar(out=rs3[:, dch, :], in0=rs3[:, dch, :], scalar1=ALPHA, scalar2=None, op0=ALU.mult),
               reads=[k.RS], writes=[k.RS])
    for t in range(8):
        ps = k.bank()
        for dch in range(16):
            k.mm(ps, ps[:, 0:32], TKF, tkf3[:, dch, t * 128:(t + 1) * 128], RWF, rwf3[:, dch, :], start=(dch == 0), stop=(dch == 15))
        k.tt('dve', LG, lg3[:, t, :], ps, ps[:, 0:32], RB, RB.ap, ALU.add)
    for t in range(8):
        MX = SMT[:, 0:8]
        k.S.op('dve', lambda e, t=t: e.max(out=SMT[:, 0:8], in_=lg3[:, t, :]), reads=[LG], writes=[SMT])
        k.ts('dve', MASK, mask3[:, t, :], LG, lg3[:, t, :], SMT[:, 3:4], None, ALU.is_ge, extra=[SMT])
        k.ts('dve', SMT, SMT[:, 8:9], SMT, SMT[:, 0:1], -1.0, None, ALU.mult)
        k.act(GATE, gate3[:, t, :], LG, lg3[:, t, :], AF.Exp, bias=SMT[:, 8:9], extra=[SMT])
        k.tt('dve', GATE, gate3[:, t, :], GATE, gate3[:, t, :], MASK, mask3[:, t, :], ALU.mult)
        k.S.op('dve', lambda e, t=t: e.reduce_sum(out=SMT[:, 9:10], in_=gate3[:, t, :], axis=AX.X), reads=[GATE], writes=[SMT])
        k.S.op('dve', lambda e: e.reciprocal(out=SMT[:, 10:11], in_=SMT[:, 9:10]), reads=[SMT], writes=[SMT])
        k.ts('dve', GATE, gate3[:, t, :], GATE, gate3[:, t, :], SMT[:, 10:11], None, ALU.mult, extra=[SMT])
    k.cp('dve', GHL, ghl4[:, :, :, 0], GATE, gate3)
    k.cp('dve', LG, lg3, GHL, ghl4[:, :, :, 0])
    k.tt('dve', LG, LG.ap, GATE, GATE.ap, LG, LG.ap, ALU.subtract)
    k.cp('dve', GHL, ghl4[:, :, :, 1], LG, lg3)
    k.cp('dve', MASKB, MASKB.ap, MASK, MASK.ap)
    CNT = k.alloc('cnt', 32)
    k.S.op('dve', lambda e: e.tensor_reduce(out=CNT.ap, in_=mask3.rearrange("p t e -> p e t"), axis=AX.X, op=ALU.add), reads=[MASK], writes=[CNT])
    pcn = k.bank()
    k.mm(pcn, pcn[:, 0:32], CF, CF[:, C_ONES:C_ONES + 128], CNT, CNT.ap)
    k.cp('dve', CNT, CNT.ap, pcn, pcn[:, 0:32])
    co = k.dout('cnt%d' % layer, [1, 32])
    k.dma('sp', co, co.ap, CNT, CNT[0:1, :])
    for t in range(8):
        ps = k.bank()
        k.mm(ps, ps[:, 0:32], CB, CB[:, C_TRI:C_TRI + 128], MASKB, maskb3[:, t, :], start=True, stop=(t == 0))
        for i in range(t):
            k.mm(ps, ps[:, 0:32], CB, CB[:, C_ONES:C_ONES + 128], MASKB, maskb3[:, i, :], start=False, stop=(i == t - 1))
        k.cp('dve', RANK, rank3[:, t, :], ps, ps[:, 0:32])
    k.release()
    PM = k.alloc('pm', 8 * CAP, BF16)
    pm3 = r3(PM.ap, CAP)
    PT = k.alloc('pt', NSB * NOWN, BF16)
    pt3 = r3(PT.ap, NOWN)
    GS = k.alloc('gs', NSB)
    GS4 = k.alloc('gs4', 2 * NSB)
    XE = k.alloc('xe', 16 * CAP, BF16)
    xe3 = r3(XE.ap, CAP)
    ACTT = k.alloc('actt', 16 * CAP, BF16)
    at3 = r3(ACTT.ap, CAP)
    YE = [k.alloc('ye%d' % i, NSB * 256, BF16) for i in range(2)]
    w1, w2, b2 = I['e_w1'], I['e_w2'], I['e_b2']
    NB = 6
    WR = [k.alloc('wr%d' % i, 16 * 256, BF16) for i in range(NB)]
    B2B1 = k.alloc('b2b', D, BF16)
    blocks = []
    for e_ in range(nexp):
        for fb_ in range(8):
            blocks.append(('g', e_, fb_))
            blocks.append(('l', e_, fb_))
        blocks.append(('b2', e_, 0))
        for db_ in range(8):
            blocks.append(('w2', e_, db_))
    wst = {'issued': 0, 'ring': 0}
    slot_of = {}

    def issue_upto(n):
        while wst['issued'] < min(n, len(blocks)):
            kind, e_, idx = blocks[wst['issued']]
            if kind == 'b2':
                k.dma('pool', B2B1, B2B1[0:1, :], b2, b2.ap[layer, e_:e_ + 1, :])
            else:
                buf = WR[wst['ring'] % NB]
                wst['ring'] += 1
                slot_of[(kind, e_, idx)] = buf
                if kind == 'g':
                    src = w1.ap[layer, e_, :, idx * 256:(idx + 1) * 256]
                elif kind == 'l':
                    src = w1.ap[layer, e_, :, D + idx * 256:D + (idx + 1) * 256]
                else:
                    src = w2.ap[layer, e_, :, idx * 256:(idx + 1) * 256]
                k.dma('pool', buf, r3(buf.ap, 256), w1 if kind != 'w2' else w2, src.rearrange("(j p) c -> p j c", p=128))
            wst['issued'] += 1

    TG = k.alloc('tg', CAP)
    TL = k.alloc('tl', CAP)
    TS_ = k.alloc('tsg', CAP)
    IOTA = CF[:, C_IOTA:C_IOTA + CAP]
    ny = 0
    issue_upto(NB)
    for e in range(nexp):
        b1e = B1E[e % 2]
        k.dma('sp', b1e, b1e.ap, I['e_b1'], I['e_b1'].ap[:, layer, e, :])
        for t in range(8):
            k.ts('dve', PM, pm3[:, t, :], CF, IOTA, rank3[:, t, e:e + 1], mask3[:, t, e:e + 1], ALU.is_equal, ALU.mult, extra=[RANK, MASK])
        for sb in range(NSB):
            for tg in range(2):
                ps = k.bank()
                for j in range(4):
                    t = tg * 4 + j
                    k.mm(ps, ps[:, j * 128:(j + 1) * 128], PM, pm3[:, t, sb * 128:(sb + 1) * 128], CB, IDB)
                k.cp('act', PT, pt3[:, sb, tg * 512:(tg + 1) * 512], ps, ps.ap)
        ps = k.bank()
        for sb in range(NSB):
            for t in range(8):
                k.mm(ps, ps[:, sb * 2:sb * 2 + 2], PM, pm3[:, t, sb * 128:(sb + 1) * 128], GHL, ghl4[:, t, e, :], start=(t == 0), stop=(t == 7))
        k.cp('act', GS4, GS4.ap, ps, ps[:, 0:2 * NSB])
        gs43 = r3(GS4.ap, 2)
        k.tt('dve', GS, GS.ap, GS4, gs43[:, :, 0], GS4, gs43[:, :, 1], ALU.add)
        for dch in range(16):
            ps = k.bank()
            for t in range(8):
                k.mm(ps, ps[:, 0:CAP], TOK, tok3[:, t, dch * 128:(dch + 1) * 128], PM, pm3[:, t, :], start=(t == 0), stop=(t == 7))
            k.cp('act' if dch % 2 else 'dve', XE, xe3[:, dch, :], ps, ps[:, 0:CAP])
        for fb in range(8):
            issue_upto(e * 25 + fb * 2 + NB)
            wg_, wl_ = slot_of[('g', e, fb)], slot_of[('l', e, fb)]
            wg3_, wl3_ = r3(wg_.ap, 256), r3(wl_.ap, 256)
            for fl in range(2):
                fch = fb * 2 + fl
                psg_, psl_ = k.bank(), k.bank()
                for dch in range(16):
                    k.mm(psg_, psg_[:, 0:CAP], wg_, wg3_[:, dch, fl * 128:(fl + 1) * 128], XE, xe3[:, dch, :], start=(dch == 0), stop=(dch == 15))
                for dch in range(16):
                    k.mm(psl_, psl_[:, 0:CAP], wl_, wl3_[:, dch, fl * 128:(fl + 1) * 128], XE, xe3[:, dch, :], start=(dch == 0), stop=(dch == 15))
                k.ts('dve', TG, TG.ap, psg_, psg_[:, 0:CAP], b1e[:, fch:fch + 1], 7.0, ALU.add, ALU.min, extra=[b1e])
                k.ts('dve', TL, TL.ap, psl_, psl_[:, 0:CAP], b1e[:, 16 + fch:17 + fch], 7.0, ALU.add, ALU.min, extra=[b1e])
                k.ts('dve', TL, TL.ap, TL, TL.ap, -7.0, 1.0, ALU.max, ALU.add)
                k.act(TS_, TS_.ap, TG, TG.ap, AF.Sigmoid, scale=1.702)
                k.tt('dve', TS_, TS_.ap, TS_, TS_.ap, TG, TG.ap, ALU.mult)
                k.tt('dve', ACTT, at3[:, fch, :], TS_, TS_.ap, TL, TL.ap, ALU.mult)
        for db in range(8):
            issue_upto(e * 25 + 17 + db + NB)
            w2_ = slot_of[('w2', e, db)]
            w23 = r3(w2_.ap, 256)
            ye = YE[ny % 2]
            ny += 1
            ye3 = r3(ye.ap, 256)
            for sb in range(NSB):
                if sb % 2 == 0:
                    ps = k.bank()
                c0 = (sb % 2) * 256
                for fch in range(16):
                    k.mm(ps, ps[:, c0:c0 + 256], ACTT, at3[:, fch, sb * 128:(sb + 1) * 128], w2_, w23[:, fch, :], start=(fch == 0), stop=False)
                k.mm(ps, ps[:, c0:c0 + 256], CB, CB[0:1, C_ONES:C_ONES + 128], B2B1, B2B1[0:1, db * 256:(db + 1) * 256], start=False, stop=True)
                k.act(ye, ye3[:, sb, :], ps, ps[:, c0:c0 + 256], AF.Copy, scale=GS[:, sb:sb + 1], extra=[GS])
            for dl in range(2):
                dch = db * 2 + dl
                for half in range(2):
                    ps = k.bank()
                    for sb in range(NSB):
                        k.mm(ps, ps.ap, ye, ye3[:, sb, dl * 128:(dl + 1) * 128], PT, pt3[:, sb, half * 512:(half + 1) * 512], start=(sb == 0), stop=(sb == NSB - 1))
                    k.stt('dve', k.RS, rs3[:, dch, half * 512:(half + 1) * 512], ps, ps.ap, modv(k, layer, 5, dch),
                          k.RS, rs3[:, dch, half * 512:(half + 1) * 512], ALU.mult, ALU.add, extra=[k.MODV])
    k.release()


def phase_B(k):
    I, CF, CB = k.I, k.CF, k.CB
    rs3 = r3(k.RS.ap, NOWN)
    G1 = k.alloc('g1', 32 * NOWN, BF16)
    k.G1 = G1
    g13 = r3(G1.ap, NOWN)
    VGS = k.dscr('s_vg', [8, 128, E_IN])
    k.mark()
    HX1 = k.alloc('hx1', 16 * NOWN, BF16)
    hx3 = r3(HX1.ap, NOWN)
    for dch in range(16):
        k.act(HX1, hx3[:, dch, :], k.RS, rs3[:, dch, :], AF.Identity, scale=mod1(k, 1, 1, dch), bias=modv(k, 1, 0, dch), extra=[k.MOD1, k.MODV])
    for dch in range(16):
        k.S.op('pool', lambda e, dch=dch: e.tensor_scalar(out=rs3[:, dch, :], in0=rs3[:, dch, :], scalar1=ALPHA, scalar2=None, op0=ALU.mult),
               reads=[k.RS], writes=[k.RS])
    MV = k.alloc('mv', 8 * 2)
    RSTDV = k.alloc('rstdv', 8)
    mv3 = r3(MV.ap, 2)
    w = I['b_w_in']
    k.mark()
    STATS = k.alloc('stats', 8 * 16 * 6)
    st4 = STATS.ap.rearrange("p (t c s) -> p t c s", t=8, c=16)
    WV = [k.alloc('wv%d' % i, 16 * 256, BF16) for i in range(2)]
    VGT = [k.alloc('vgt%d' % i, 256) for i in range(2)]
    VGB = [k.alloc('vgb%d' % i, 256, BF16) for i in range(2)]
    n = 0
    def load_wv(cb_):
        wv_ = WV[cb_ % 2]
        k.dma('pool', wv_, r3(wv_.ap, 256), w, w.ap[:, E_IN + cb_ * 256:E_IN + (cb_ + 1) * 256].rearrange("(j p) c -> p j c", p=128))
    load_wv(0)
    for cb in range(16):
        wv = WV[cb % 2]
        wv3 = r3(wv.ap, 256)
        if cb + 1 < 16:
            load_wv(cb + 1)
        for t in range(8):
            ps = k.bank()
            for dch in range(16):
                k.mm(ps, ps[:, 0:256], HX1, hx3[:, dch, t * 128:(t + 1) * 128], wv, wv3[:, dch, :], start=(dch == 0), stop=(dch == 15))
            vgt, vgb = VGT[n % 2], VGB[n % 2]
            n += 1
            k.act(vgt, vgt.ap, ps, ps[:, 0:256], AF.Gelu)
            k.S.op('dve', lambda e, t=t, cb=cb, vgt=vgt: e.bn_stats(out=st4[:, t, cb, :], in_=vgt.ap), reads=[vgt], writes=[STATS])
            k.cp('pool', vgb, vgb.ap, vgt, vgt.ap)
            k.dma('sp', VGS, VGS.ap[t, :, cb * 256:(cb + 1) * 256], vgb, vgb.ap)
    for t in range(8):
        k.S.op('dve', lambda e, t=t: e.bn_aggr(out=mv3[:, t, :], in_=st4[:, t, :, :]), reads=[STATS], writes=[MV])
    k.act(RSTDV, RSTDV.ap, MV, mv3[:, :, 1], AF.Sqrt, bias=k.EPS[:, 0:1], extra=[k.EPS])
    k.S.op('dve', lambda e: e.reciprocal(out=RSTDV.ap, in_=RSTDV.ap), reads=[RSTDV], writes=[RSTDV])
    k.release()
    WSB = k.alloc('wsb', 8 * 128, BF16)
    BSB = k.alloc('bsb', 8 * 128)
    k.mark()
    WSf = k.alloc('wsf', 8 * 128)
    k.dma('sp', WSf, WSf.ap, I['b_wsT'], I['b_wsT'].ap.rearrange("p g t -> p (g t)"))
    k.cp('dve', WSB, WSB.ap, WSf, WSf.ap)
    k.release()
    k.dma('sp', BSB, BSB.ap, I['b_bs'], I['b_bs'].ap.rearrange("g t -> (g t)").partition_broadcast(128))
    wsb3, bsb3 = r3(WSB.ap, 128), r3(BSB.ap, 128)
    VGG = [k.alloc('vgg', 8 * 512, BF16)] * 2
    LNG = [k.alloc('lng', 512)] * 2
    LNB = [k.alloc('lnb', 512)] * 2
    TN = [k.alloc('tn%d' % i, 512) for i in range(2)]
    WU = [k.alloc('wu%d' % i, 16 * 128, BF16) for i in range(2)]
    UT = [k.alloc('ut', NOWN, BF16)] * 2
    TM = k.alloc('tm', NOWN)
    nn = 0
    for g in range(8):
        vgg = VGG[g % 2]
        vgg3 = r3(vgg.ap, 512)
        lng, lnb = LNG[g % 2], LNB[g % 2]
        k.dma('sp', vgg, vgg3, VGS, VGS.ap[:, :, g * 512:(g + 1) * 512].rearrange("c p n -> p c n"))
        k.dma('sp', lng, lng.ap, I['b_lng'], I['b_lng'].ap[0, g * 512:(g + 1) * 512].partition_broadcast(128))
        k.dma('sp', lnb, lnb.ap, I['b_lnb'], I['b_lnb'].ap[0, g * 512:(g + 1) * 512].partition_broadcast(128))
        for c in range(8):
            tn = TN[c % 2]
            k.ts('dve', tn, tn.ap, vgg, vgg3[:, c, :], mv3[:, c, 0:1], RSTDV[:, c:c + 1], ALU.subtract, ALU.mult, extra=[MV, RSTDV])
            k.tt('pool', tn, tn.ap, tn, tn.ap, lng, lng.ap, ALU.mult)
            k.tt('pool', vgg, vgg3[:, c, :], tn, tn.ap, lnb, lnb.ap, ALU.add)
        for cl in range(4):
            cc = g * 4 + cl
            wu = WU[cc % 2]
            wu3 = r3(wu.ap, 128)
            if cc == 0:
                k.dma('pool', wu, wu3, w, w.ap[:, 0:128].rearrange("(j p) c -> p j c", p=128))
            if cc + 1 < 32:
                wun = WU[(cc + 1) % 2]
                k.dma('pool', wun, r3(wun.ap, 128), w, w.ap[:, (cc + 1) * 128:(cc + 2) * 128].rearrange("(j p) c -> p j c", p=128))
            ut = UT[cc % 2]
            for half in range(2):
                ps = k.bank()
                for dch in range(16):
                    k.mm(ps, ps.ap, wu, wu3[:, dch, :], HX1, hx3[:, dch, half * 512:(half + 1) * 512], start=(dch == 0), stop=(dch == 15))
                k.act(ut, ut[:, half * 512:(half + 1) * 512], ps, ps.ap, AF.Gelu)
            for half in range(2):
                ps = k.bank()
                for j in range(4):
                    c = half * 4 + j
                    k.mm(ps, ps[:, j * 128:(j + 1) * 128], vgg, vgg3[:, c, cl * 128:(cl + 1) * 128], WSB, wsb3[:, g, :])
                k.tt('dve', TM, r3(TM[:, half * 512:(half + 1) * 512], 128), ps, r3(ps.ap, 128), BSB,
                     bsb3[:, g, :].unsqueeze(1).broadcast_to([128, 4, 128]), ALU.add)
            k.tt('pool', G1, g13[:, cc, :], TM, TM.ap, ut, ut.ap, ALU.mult)
    k.release()


def phase_out(k, OUT):
    CF = k.CF
    rs3 = r3(k.RS.ap, NOWN)
    k.mark()
    OT = [k.alloc('ot%d' % i, D) for i in range(2)]
    for t in range(8):
        ot = OT[t % 2]
        for g in range(4):
            ps = k.bank()
            for i in range(4):
                dch = g * 4 + i
                k.mm(ps, ps[:, i * 128:(i + 1) * 128], k.RS, rs3[:, dch, t * 128:(t + 1) * 128], CF, CF[:, C_ID:C_ID + 128])
            k.cp('act' if g % 2 else 'dve', ot, ot[:, g * 512:(g + 1) * 512], ps, ps.ap)
        k.dma('sp', OUT, OUT.ap[t * 128:(t + 1) * 128, :], ot, ot.ap)
    k.release()


def kernel(**inputs):
    if 'nc' not in _CACHE:
        _CACHE['nc'] = build_program()
    nc = _CACHE['nc']
    names = _CACHE['names']
    sh = prep_shared(inputs)
    in_maps = []
    for core in range(8):
        pc = prep_core(inputs, core, sh)
        allin = {**sh, **pc}
        in_maps.append({n: allin[n] for n in names})
    res = run_bass_kernel_spmd(nc, in_maps, core_ids=list(range(8)))
    out = np.zeros((4, 2048, D), np.float32)
    for core in range(8):
        b, half = core // 2, core % 2
        yl = np.asarray(res.results[core]['yl'], np.float32)
        try:
            print('[moe-load] core', core, 'max tokens/expert L0', float(np.max(res.results[core]['cnt0'])),
                  'L1', float(np.max(res.results[core]['cnt1'])), 'cap', CAP, flush=True)
        except Exception:
            pass
        if half == 0:
            out[b, 0:NOWN] = yl
        else:
            out[b, NOWN:2048] = yl[::-1]
    return out
```

```python
from contextlib import ExitStack
import numpy as np
import concourse.bass as bass
import concourse.mybir as mybir
from concourse.bass_utils import run_bass_kernel_spmd

F32 = mybir.dt.float32
BF16 = mybir.dt.bfloat16
AF = mybir.ActivationFunctionType
ALU = mybir.AluOpType
AX = mybir.AxisListType

D = 2048
NT = 2304
NOWN = 1024
E_IN = 4096
NEXP = 32
NCC_DBG = 2
CAP = 384
NSB = CAP // 128
ALPHA = 4.0 ** 0.25
LN_EPS = 1e-5
NEG = -1.0e30
ARENA = 53000

ENG = ['pe', 'act', 'dve', 'pool', 'sp']
NDSEM = 8
_CACHE = {}


class Buf:
    __slots__ = ('ap', 'name', 'w', 'rd')

    def __init__(self, ap, name=''):
        self.ap = ap
        self.name = name
        self.w = None
        self.rd = []

    def __getitem__(self, k):
        return self.ap[k]


class Sched:
    def __init__(self, nc, es):
        self.nc = nc
        self.prog = {e: [] for e in ENG}
        self.esem = {e: es.enter_context(nc.semaphore('s_' + e)) for e in ENG}
        self.ecnt = {e: 0 for e in ENG}
        self.seen = {e: {} for e in ENG}
        self.dq = ['sp', 'act', 'pool']
        self.dsem = {q: [es.enter_context(nc.semaphore('d_%s%d' % (q, i))) for i in range(NDSEM)] for q in self.dq}
        self.dcnt = {q: [0] * NDSEM for q in self.dq}
        self.dn = {q: 0 for q in self.dq}
        self.nins = 0

    def _semof(self, key):
        if key[0] == 'E':
            return self.esem[key[1]]
        return self.dsem[key[1][0]][key[1][1]]

    def _waits(self, eng, deps):
        best = {}
        for t in deps:
            if t is None:
                continue
            key = (t[0], t[1])
            if t[2] > best.get(key, 0):
                best[key] = t[2]
        for key, v in best.items():
            if self.seen[eng].get(key, 0) >= v:
                continue
            self.seen[eng][key] = v
            self.prog[eng].append(('wait', self._semof(key), v))

    def op(self, eng, fn, reads=(), writes=()):
        deps = set()
        for b in reads:
            if b.w is not None and not (eng == 'pe' and b.w[0] == 'E' and b.w[1] == 'pe'):
                deps.add(b.w)
        for b in writes:
            if b.w is not None and not (b.w[0] == 'E' and b.w[1] == eng):
                deps.add(b.w)
            for t in b.rd:
                if not (t[0] == 'E' and t[1] == eng):
                    deps.add(t)
        self._waits(eng, deps)
        self.ecnt[eng] += 1
        tok = ('E', eng, self.ecnt[eng])
        self.prog[eng].append(('ins', fn, self.esem[eng]))
        self.nins += 1
        wset = set(id(b) for b in writes)
        for b in reads:
            if id(b) in wset:
                continue
            b.rd = [t for t in b.rd if not (t[0] == 'E' and t[1] == eng)]
            b.rd.append(tok)
        for b in writes:
            b.w = tok
            b.rd = []
        return tok

    def dma(self, q, out_buf, out_ap, in_buf, in_ap):
        deps = set()
        if in_buf.w is not None:
            deps.add(in_buf.w)
        if out_buf.w is not None:
            deps.add(out_buf.w)
        deps.update(out_buf.rd)
        slot = self.dn[q] % NDSEM
        self.dn[q] += 1
        self.dcnt[q][slot] += 1
        val = 16 * self.dcnt[q][slot]
        if val > 16:
            deps.add(('D', (q, slot), val - 16))
        self._waits(q, deps)
        sem = self.dsem[q][slot]

        def fn(e, out_ap=out_ap, in_ap=in_ap):
            return e.dma_start(out=out_ap, in_=in_ap)
        self.prog[q].append(('dma', fn, sem))
        self.nins += 1
        tok = ('D', (q, slot), val)
        if in_buf.w is not None or True:
            in_buf.rd = [t for t in in_buf.rd if not (t[0] == 'D' and t[1] == (q, slot))]
            in_buf.rd.append(tok)
        out_buf.w = tok
        out_buf.rd = []
        return tok

    def barrier(self):
        toks = set()
        for e in ENG:
            if self.ecnt[e] > 0:
                toks.add(('E', e, self.ecnt[e]))
        for q in self.dq:
            for s in range(NDSEM):
                if self.dcnt[q][s] > 0:
                    toks.add(('D', (q, s), 16 * self.dcnt[q][s]))
        for e in ENG:
            self._waits(e, [t for t in toks if not (t[0] == 'E' and t[1] == e)])

    def emit(self, block):
        prog = self.prog

        def replay(e, items):
            for it in items:
                if it[0] == 'wait':
                    e.wait_ge(it[1], it[2])
                elif it[0] == 'ins':
                    it[1](e).then_inc(it[2], 1)
                else:
                    it[1](e).then_inc(it[2], 16)

        @block.tensor
        def _(e):
            replay(e, prog['pe'])

        @block.scalar
        def _(e):
            replay(e, prog['act'])

        @block.vector
        def _(e):
            replay(e, prog['dve'])

        @block.gpsimd
        def _(e):
            replay(e, prog['pool'])

        @block.sync
        def _(e):
            replay(e, prog['sp'])


C_ID = 0
C_ONES = 128
C_U = 256
C_L = 384
C_TRI = 512
C_BM = 640
C_IOTA = 768
C_PIDX = 1280
NCONST = 1282


def make_consts():
    c = np.zeros((128, NCONST), np.float32)
    p = np.arange(128)
    c[:, C_ID:C_ID + 128] = np.eye(128)
    c[:, C_ONES:C_ONES + 128] = 1.0
    c[:, C_U:C_U + 128] = (p[:, None] <= p[None, :])
    c[:, C_L:C_L + 128] = (p[:, None] >= p[None, :])
    c[:, C_TRI:C_TRI + 128] = (p[:, None] < p[None, :])
    c[:, C_BM:C_BM + 128] = (p[:, None] // 4 == (p[None, :] // 4))
    c[:, C_IOTA:C_IOTA + 512] = np.arange(512)[None, :]
    c[:, C_PIDX] = p
    c[:, C_PIDX + 1] = p + 128
    return c


def colmajor(v, n=None):
    v = np.asarray(v, np.float32).reshape(-1, 128)
    return np.ascontiguousarray(v.T)


IN_SHAPES = {
    'xl': [2048, D], 'ctxl': [256, D], 'cvec': [D, 2], 'consts': [128, NCONST],
    'mod_w': [2, D, 6 * D], 'modb': [128, 192], 'lnp': [128, 2, 4, 16],
    'a_w_in': [D, 2 * E_IN], 'a_cw': [128, 32, 9], 'a_vec': [128, 3, 32],
    'a_w4': [128, 3, 32, 4], 'a_w4t': [128, 3, 32, 4], 'a_wg': [128, 96, 16], 'a_bg': [4, 4],
    'a_w_out': [E_IN, D],
    'b_w_in': [D, 2 * E_IN], 'b_lng': [1, E_IN], 'b_lnb': [1, E_IN], 'b_wsT': [128, 8, 128], 'b_bs': [8, 128],
    'b_w_out': [E_IN, D],
    'r_w': [2, D, NEXP], 'r_b': [2, NEXP], 'e_w1': [2, NEXP, D, 2 * D], 'e_b1': [128, 2, NEXP, 32],
    'e_w2': [2, NEXP, D, D], 'e_b2': [2, NEXP, D],
}


class LazyIn:
    def __init__(self, k):
        self.k = k
        self.d = {}

    def __getitem__(self, name):
        if name not in self.d:
            self.d[name] = self.k.din(name, IN_SHAPES[name])
        return self.d[name]


class KB:
    def __init__(self, nc, es):
        self.nc = nc
        self.es = es
        self.S = Sched(nc, es)
        self.arena = es.enter_context(nc.sbuf_tensor("arena", [128, ARENA], F32))
        self.psum = es.enter_context(nc.psum_tensor("psum", [128, 4096], F32))
        self.pb = [Buf(self.psum[:, i * 512:(i + 1) * 512], 'pb%d' % i) for i in range(8)]
        self.top = 0
        self.hi = ARENA
        self.marks = []
        self.pbi = 0
        self.dbg = {}

    def alloc(self, name, cols, dt=F32):
        n32 = cols if dt == F32 else (cols + 1) // 2
        assert self.top + n32 <= self.hi, (name, self.top, n32, self.hi)
        ap = self.arena[:, self.top:self.top + n32]
        self.top += n32
        if dt != F32:
            ap = ap.bitcast(dt)
        return Buf(ap, name)

    def alloc_hi(self, name, cols):
        self.hi -= cols
        assert self.hi >= self.top, (name, self.top, self.hi)
        return Buf(self.arena[:, self.hi:self.hi + cols], name)

    def mark(self):
        self.marks.append(self.top)

    def release(self):
        self.S.barrier()
        self.top = self.marks.pop()

    def din(self, name, shape, dt=F32):
        return Buf(self.nc.dram_tensor(name, list(shape), dt, kind="ExternalInput").ap(), name)

    def dout(self, name, shape, dt=F32):
        return Buf(self.nc.dram_tensor(name, list(shape), dt, kind="ExternalOutput").ap(), name)

    def dscr(self, name, shape, dt=BF16):
        return Buf(self.nc.dram_tensor(name, list(shape), dt, kind="Internal").ap(), name)

    def bank(self, lo=0, hi=8):
        b = self.pb[lo + self.pbi % (hi - lo)]
        self.pbi += 1
        return b

    def mm(self, ob, oap, lb, lap, rb, rap, start=True, stop=True):
        self.S.op('pe', lambda e: e.matmul(oap, lhsT=lap, rhs=rap, start=start, stop=stop),
                  reads=[lb, rb], writes=[ob])

    def act(self, ob, oap, ib, iap, func, scale=1.0, bias=0.0, extra=()):
        self.S.op('act', lambda e: e.activation(out=oap, in_=iap, func=func, scale=scale, bias=bias),
                  reads=[ib] + list(extra), writes=[ob])

    def ts(self, eng, ob, oap, ib, iap, s1, s2, op0, op1=None, extra=()):
        if op1 is None:
            f = lambda e: e.tensor_scalar(out=oap, in0=iap, scalar1=s1, scalar2=None, op0=op0)
        else:
            f = lambda e: e.tensor_scalar(out=oap, in0=iap, scalar1=s1, scalar2=s2, op0=op0, op1=op1)
        self.S.op(eng, f, reads=[ib] + list(extra), writes=[ob])

    def tt(self, eng, ob, oap, ab, aap, bb, bap, op):
        self.S.op(eng, lambda e: e.tensor_tensor(out=oap, in0=aap, in1=bap, op=op), reads=[ab, bb], writes=[ob])

    def stt(self, eng, ob, oap, ab, aap, sc, bb, bap, op0, op1, extra=()):
        self.S.op(eng, lambda e: e.scalar_tensor_tensor(out=oap, in0=aap, scalar=sc, in1=bap, op0=op0, op1=op1),
                  reads=[ab, bb] + list(extra), writes=[ob])

    def cp(self, eng, ob, oap, ib, iap):
        if eng == 'act':
            self.S.op('act', lambda e: e.copy(out=oap, in_=iap), reads=[ib], writes=[ob])
        else:
            self.S.op(eng, lambda e: e.tensor_copy(out=oap, in_=iap), reads=[ib], writes=[ob])

    def dma(self, q, ob, oap, ib, iap):
        self.S.dma(q, ob, oap, ib, iap)

    def debug_out(self, name, buf, ap, shape, dt=F32):
        o = self.dout(name, shape, dt)
        self.dma('sp', o, o.ap, buf, ap)
        self.dbg[name] = o


def r3(ap, b):
    return ap.rearrange("p (a b) -> p a b", b=b)


def build_program(stop_after=None, dbg=False):
    nc = bass.Bass("TRN2", target_bir_lowering=False)
    es = ExitStack()
    k = KB(nc, es)
    S = k.S
    I = LazyIn(k)
    OUT = k.dout('yl', [NOWN, D])
    k.I = I

    CF = k.alloc('consts', NCONST)
    k.dma('sp', CF, CF.ap, I['consts'], I['consts'].ap)
    CB = k.alloc('constsb', 768, BF16)
    k.cp('dve', CB, CB.ap, CF, CF[:, 0:768])
    k.CF, k.CB = CF, CB
    MODV = k.alloc('modv', 192 * 2)
    MOD1 = k.alloc('mod1', 192 * 2)
    LNP = k.alloc('lnp', 2 * 4 * 16)
    k.dma('sp', LNP, LNP.ap, I['lnp'], I['lnp'].ap.rearrange("p a b c -> p (a b c)"))
    k.MODV, k.MOD1, k.LNP = MODV, MOD1, LNP

    k.EPS = k.alloc('eps', 1)
    k.S.op('dve', lambda e: e.memset(k.EPS.ap, LN_EPS), writes=[k.EPS])
    k.LN32 = k.alloc('ln32', 1)
    k.S.op('dve', lambda e: e.memset(k.LN32.ap, -float(np.log(32.0))), writes=[k.LN32])
    phase_mod(k)
    k.mark()
    k.GTM = k.alloc('gtm', 288)
    k.COLS = k.alloc('cols', 288)
    k.INTB = k.alloc('intb', 144)
    if stop_after == 'mod':
        k.debug_out('d_modv', MODV, MODV.ap, [128, 384])
        return finish(k)
    k.mark()
    phase_hx0(k)
    if stop_after == 'A':
        phase_A(k, ncc=NCC_DBG, dbg_cc=NCC_DBG - 1)
        k.debug_out("d_g", k.Gdbg, k.Gdbg.ap, [128, 1024], BF16)
        k.debug_out('d_gtm', k.GTM, k.GTM.ap, [128, 288])
        k.mark()
        TB = k.alloc('dbgt', 18 * 256, BF16)
        for (nm, scr) in (('d_ktok', k.KTOK), ('d_vtok', k.VTOK)):
            k.dma('sp', TB, r3(TB.ap, 256), scr, scr.ap[:, :, 0:256].rearrange("t p c -> p t c"))
            o = k.dout(nm, [18, 128, 256], BF16)
            k.dma('sp', o, o.ap.rearrange("t p c -> p t c"), TB, r3(TB.ap, 256))
        for (nm, scr) in (('d_qt', k.QT), ('d_kt', k.KT), ('d_szo', k.SZO)):
            k.dma('sp', TB, r3(TB.ap[:, 0:2048], 1024), scr, scr.ap[0:2].rearrange("t p c -> p t c"))
            o = k.dout(nm, [2, 128, NOWN], BF16)
            k.dma('sp', o, o.ap.rearrange("t p c -> p t c"), TB, r3(TB.ap[:, 0:2048], 1024))
        k.release()
        return finish(k)
    if stop_after == 'S':
        phase_A(k, ncc=8)
        k.release()
        phase_G(k)
        k.debug_out('d_cols', k.COLS, k.COLS.ap, [128, 288])
        k.debug_out('d_intb', k.INTB, k.INTB.ap, [128, 144])
        phase_scan(k, heads=(0,), dbg_hs=0)
        k.debug_out('d_gt', k.GT, k.GT[:, 0:8 * NOWN], [128, 8 * NOWN], BF16)
        return finish(k)
    phase_A(k)
    k.release()
    phase_G(k)
    k.mark()
    phase_scan(k)
    k.RS = k.alloc_hi('rs', 16 * NOWN)
    build_rs0(k)
    phase_wout(k, k.GT, 'a_w_out', 0)
    k.release()
    k.release()
    ln_fm(k, 0, 0)
    phase_moe(k, 0)
    ln_fm(k, 0, 2)
    k.mark()
    phase_B(k)
    phase_wout(k, k.G1, 'b_w_out', 1)
    k.release()
    ln_fm(k, 1, 0)
    phase_moe(k, 1)
    ln_fm(k, 1, 2)
    phase_out(k, OUT)
    return finish(k)


def finish(k):
    _CACHE['names'] = list(k.I.d.keys())
    k.S.barrier()
    with k.nc.Block() as block:
        k.S.emit(block)
    k.es.close()
    return k.nc


def mod_col(layer, part, dch):
    return (layer * 6 + part) * 16 + dch


def phase_mod(k):
    I, S = k.I, k.S
    k.mark()
    CV = k.alloc('cv', 32)
    SCV = k.alloc('scv', 32)
    MB = k.alloc('modb', 192)
    k.dma('sp', CV, r3(CV.ap, 2), I['cvec'], I['cvec'].ap.rearrange("(j p) n -> p j n", p=128))
    k.dma('sp', MB, MB.ap, I['modb'], I['modb'].ap)
    k.act(SCV, SCV.ap, CV, CV.ap, AF.Silu)
    scv3 = r3(SCV.ap, 2)
    MW = [k.alloc('mw%d' % i, 16 * 512) for i in range(2)]
    ROW = k.alloc('modrow', 2 * 6 * D)
    n = 0
    for layer in range(2):
        for blk in range(24):
            mw = MW[n % 2]
            n += 1
            src = k.I['mod_w'].ap[layer, :, blk * 512:(blk + 1) * 512].rearrange("(j p) c -> p j c", p=128)
            k.dma('sp' if n % 2 else 'act', mw, r3(mw.ap, 512), I['mod_w'], src)
            mw3 = r3(mw.ap, 512)
            pr = k.bank(1, 8)
            for dch in range(16):
                k.mm(pr, pr[0:2, :], SCV, scv3[:, dch, :], mw, mw3[:, dch, :], start=(dch == 0), stop=(dch == 15))
            c0 = layer * 6 * D + blk * 512
            k.cp('act' if n % 2 else 'dve', ROW, ROW[0:2, c0:c0 + 512], pr, pr[0:2, :])
    ps = k.pb[0]
    ps3 = r3(ps.ap[:, 0:384], 2)
    for j in range(192):
        k.mm(ps, ps3[:, j, :], ROW, ROW[0:2, j * 128:(j + 1) * 128], k.CF, k.CF[0:2, C_ID:C_ID + 2])
    mv3 = r3(k.MODV.ap, 2)
    m13 = r3(k.MOD1.ap, 2)
    for r in range(2):
        k.tt('dve', k.MODV, mv3[:, :, r], ps, ps3[:, :, r], MB, MB.ap, ALU.add)
    k.ts('dve', k.MOD1, k.MOD1.ap, k.MODV, k.MODV.ap, 1.0, None, ALU.add)
    k.release()


def prep_shared(inp):
    f = lambda a: np.ascontiguousarray(np.asarray(a, np.float32))
    sh = {}
    sh['consts'] = make_consts()
    sh['mod_w'] = f(inp['mod_w'])
    sh['modb'] = np.concatenate([colmajor(inp['mod_b'][0]), colmajor(inp['mod_b'][1])], axis=1)
    lnp = np.zeros((128, 2, 4, 16), np.float32)
    for l in range(2):
        for i, nm in enumerate(['ln1_g', 'ln1_b', 'ln2_g', 'ln2_b']):
            lnp[:, l, i, :] = colmajor(inp[nm][l])
    sh['lnp'] = lnp
    sh['a_w_in'] = f(inp['a_w_in'][0])
    sh['a_vec'] = np.stack([colmajor(inp['a_conv_b'][0]), colmajor(inp['a_norm_w'][0]), colmajor(inp['a_skip'][0])], axis=1)
    w4 = np.zeros((128, 3, 32, 4), np.float32)
    w4t = np.zeros((128, 3, 32, 4), np.float32)
    for i, nm in enumerate(['a_w_q', 'a_w_k', 'a_w_v']):
        w = f(inp[nm][0]).reshape(32, 32, 4, 4)
        w4[:, i] = w.transpose(1, 2, 0, 3).reshape(128, 32, 4)
        w4t[:, i] = w.transpose(1, 3, 0, 2).reshape(128, 32, 4)
    sh['a_w4'] = w4
    sh['a_w4t'] = w4t
    sh['a_w_out'] = f(inp['a_w_out'][0])
    sh['b_w_in'] = f(inp['b_w_in'][0])
    sh['b_lng'] = f(inp['b_ln_g'][0]).reshape(1, E_IN)
    sh['b_lnb'] = f(inp['b_ln_b'][0]).reshape(1, E_IN)
    sh['b_w_out'] = f(inp['b_w_out'][0])
    sh['r_w'] = f(inp['r_w'])
    sh['r_b'] = f(inp['r_b'])
    sh['e_w1'] = f(inp['e_w1'])
    sh['e_w2'] = f(inp['e_w2'])
    sh['e_b2'] = f(inp['e_b2'])
    b1 = f(inp['e_b1']).reshape(2, NEXP, 32, 128)
    sh['e_b1'] = np.ascontiguousarray(b1.transpose(3, 0, 1, 2))
    return sh


def prep_core(inp, core, sh):
    f = lambda a: np.ascontiguousarray(np.asarray(a, np.float32))
    b, half = core // 2, core % 2
    d = {}
    x = np.asarray(inp['x'][b], np.float32)
    ctx = np.asarray(inp['ctx'][b], np.float32)
    cw = np.asarray(inp['a_conv_w'][0], np.float32)
    wg = np.asarray(inp['a_w_gate'][0], np.float32)
    bg = np.asarray(inp['a_b_gate'][0], np.float32)
    ws = np.asarray(inp['b_w_s'][0], np.float32)
    bs = np.asarray(inp['b_b_s'][0], np.float32)
    if half == 1:
        x = x[::-1]
        ctx = ctx[::-1]
        cw = cw[::-1, ::-1]
        wg = np.concatenate([wg[:, 8:16], wg[:, 0:8]], axis=1)
        bg = np.concatenate([bg[8:16], bg[0:8]])
        ws = ws[:, ::-1, ::-1]
        bs = bs[:, ::-1]
    d['xl'] = f(x)
    d['ctxl'] = f(ctx)
    d['cvec'] = f(np.stack([np.asarray(inp['c'][b], np.float32), np.asarray(inp['c_ctx'], np.float32)], axis=1))
    d['a_cw'] = f(cw.reshape(9, 32, 128).transpose(2, 1, 0))
    d['a_wg'] = f(wg.reshape(96, 128, 16).transpose(1, 0, 2))
    d['a_bg'] = f(bg.reshape(4, 4).T)
    d['b_wsT'] = f(ws.transpose(2, 0, 1))
    d['b_bs'] = f(bs)
    return d


def phase_hx0(k):
    I = k.I
    CF = k.CF
    HXT = k.alloc('hxt', 16 * NT, BF16)
    k.HXT = HXT
    hx3 = r3(HXT.ap, NT)
    mv3 = r3(k.MODV.ap, 2)
    m13 = r3(k.MOD1.ap, 2)
    k.mark()
    XT = [k.alloc('xt%d' % i, D) for i in range(2)]
    for tt in range(18):
        xt = XT[tt % 2]
        if tt < 2:
            sb, src, r = I['ctxl'], I['ctxl'].ap[tt * 128:(tt + 1) * 128, :], 1
        else:
            sb, src, r = I['xl'], I['xl'].ap[(tt - 2) * 128:(tt - 1) * 128, :], 0
        k.dma('sp', xt, xt.ap, sb, src)
        for g in range(4):
            ps = k.bank()
            for i in range(4):
                dch = g * 4 + i
                k.mm(ps, ps[:, i * 128:(i + 1) * 128], xt, xt[:, dch * 128:(dch + 1) * 128], CF, CF[:, C_ID:C_ID + 128])
            for i in range(4):
                dch = g * 4 + i
                k.act(HXT, hx3[:, dch, tt * 128:(tt + 1) * 128], ps, ps[:, i * 128:(i + 1) * 128], AF.Identity,
                      scale=m13[:, mod_col(0, 1, dch), r:r + 1], bias=mv3[:, mod_col(0, 0, dch), r:r + 1],
                      extra=[k.MOD1, k.MODV])
    k.release()


TBLK = [(0, 512), (512, 512), (1024, 512), (1536, 512), (2048, 256)]


def phase_A(k, ncc=32, dbg_cc=None):
    I, CF, CB = k.I, k.CF, k.CB
    HXT = k.HXT
    hx3 = r3(HXT.ap, NT)
    k.QT = k.dscr('s_qt', [32, 128, NOWN])
    k.KT = k.dscr('s_kt', [32, 128, NOWN])
    k.KTOK = k.dscr('s_ktok', [18, 128, E_IN])
    k.VTOK = k.dscr('s_vtok', [18, 128, E_IN])
    k.XCO = k.dscr('s_xco', [32, 128, NOWN])
    k.SZO = k.dscr('s_szo', [32, 128, NOWN])
    GTM = k.GTM
    k.mark()
    CW = k.alloc('cw', 32 * 9)
    AV = k.alloc('avec', 96)
    k.dma('sp', CW, CW.ap, I['a_cw'], I['a_cw'].ap.rearrange("p a b -> p (a b)"))
    k.dma('sp', AV, AV.ap, I['a_vec'], I['a_vec'].ap.rearrange("p a b -> p (a b)"))
    cw3 = r3(CW.ap, 9)
    av3 = r3(AV.ap, 32)
    BD = k.alloc('bd', 3 * 32 * 128, BF16)
    bd4 = BD.ap.rearrange("p (i c m) -> p i c m", i=3, c=32)
    G = k.alloc('g', 32 * 32, BF16)
    g3 = r3(G.ap, 32)
    k.Gdbg = G
    k.mark()
    W4 = k.alloc('w4', 384)
    W4T = k.alloc('w4t', 384)
    k.dma('sp', W4, W4.ap, I['a_w4'], I['a_w4'].ap.rearrange("p a b c -> p (a b c)"))
    k.dma('sp', W4T, W4T.ap, I['a_w4t'], I['a_w4t'].ap.rearrange("p a b c -> p (a b c)"))
    WGf = k.alloc('wgf', 96 * 16)
    k.dma('sp', WGf, WGf.ap, I['a_wg'], I['a_wg'].ap.rearrange("p a b -> p (a b)"))
    WG = k.alloc('wg', 96 * 16, BF16)
    k.cp('dve', WG, WG.ap, WGf, WGf.ap)
    wg3 = r3(WG.ap, 16)
    BDT = [k.alloc('bdt%d' % i, 3 * 128, BF16) for i in range(2)]
    bm3 = r3(CF[:, C_BM:C_BM + 128], 4)
    w44 = W4.ap.rearrange("p (i c o) -> p i c o", i=3, c=32)
    w4t4 = W4T.ap.rearrange("p (i c o) -> p i c o", i=3, c=32)
    gps = [k.pb[0], k.pb[1]]
    for cc in range(32):
        bdt = BDT[cc % 2]
        bdt3 = r3(bdt.ap, 128)
        for i in range(3):
            eng = 'dve' if (i + cc) % 2 == 0 else 'pool'
            k.tt(eng, BD, r3(bd4[:, i, cc, :], 4), CF, bm3, W4,
                 w44[:, i, cc, :].unsqueeze(1).broadcast_to([128, 32, 4]), ALU.mult)
            k.tt(eng, bdt, r3(bdt3[:, i, :], 4), CF, bm3, W4T,
                 w4t4[:, i, cc, :].unsqueeze(1).broadcast_to([128, 32, 4]), ALU.mult)
        ps = gps[cc // 16]
        c0 = (cc % 16) * 32
        k.mm(ps, ps[:, c0:c0 + 16], bdt, bdt3[:, 0, :], WG, wg3[:, cc, :], start=True, stop=False)
        k.mm(ps, ps[:, c0:c0 + 16], bdt, bdt3[:, 1, :], WG, wg3[:, 32 + cc, :], start=False, stop=True)
        k.mm(ps, ps[:, c0 + 16:c0 + 32], bdt, bdt3[:, 2, :], WG, wg3[:, 64 + cc, :], start=True, stop=True)
    for hb in range(2):
        k.cp('dve', G, G[:, hb * 512:(hb + 1) * 512], gps[hb], gps[hb].ap)
    k.release()

    XM = [k.alloc('xm%d' % i, NT) for i in range(2)]
    ACC = k.alloc('acc', NT)
    XMB = [k.alloc('xmb%d' % i, NT, BF16) for i in range(2)]
    XC = [k.alloc('xc%d' % i, NT, BF16) for i in range(2)]
    QTS = k.alloc('qts', NOWN, BF16)
    KTS = k.alloc('kts', NOWN, BF16)
    SZS = k.alloc('szs', NOWN, BF16)
    KS = k.alloc('ks', 18 * 128, BF16)
    VS = k.alloc('vs', 18 * 128, BF16)
    WI = [k.alloc('wi%d' % i, 16 * 128, BF16) for i in range(3)]
    WZ = [k.alloc('wz%d' % i, 16 * 128, BF16) for i in range(2)]
    k.pbi = 0
    awin = I['a_w_in']

    def load_w(t, col0):
        k.dma('pool', t, r3(t.ap, 128), awin, awin.ap[:, col0:col0 + 128].rearrange("(j p) c -> p j c", p=128))

    load_w(WI[0], 0)
    load_w(WZ[0], E_IN)
    def stage1(cc):
        wi, wz = WI[cc % 3], WZ[cc % 2]
        wi3, wz3 = r3(wi.ap, 128), r3(wz.ap, 128)
        if cc + 1 < ncc:
            load_w(WI[(cc + 1) % 3], (cc + 1) * 128)
            load_w(WZ[(cc + 1) % 2], E_IN + (cc + 1) * 128)
        xm, xmb, xc = XM[cc % 2], XMB[cc % 2], XC[cc % 2]
        for (t0, tn) in TBLK:
            ps = k.bank(0, 7)
            for dch in range(16):
                k.mm(ps, ps[:, 0:tn], wi, wi3[:, dch, :], HXT, hx3[:, dch, t0:t0 + tn], start=(dch == 0), stop=(dch == 15))
            k.cp('act', xm, xm[:, t0:t0 + tn], ps, ps[:, 0:tn])
        k.cp('pool', xmb, xmb.ap, xm, xm.ap)
        ce = 'dve'
        k.ts(ce, ACC, ACC.ap, xm, xm.ap, cw3[:, cc, 4:5], None, ALU.mult, extra=[CW])
        xg = r3(xm.ap[:, 256:NT], 64)
        ag = r3(ACC.ap[:, 256:NT], 64)
        for ky in range(3):
            for kx in range(3):
                if ky == 1 and kx == 1:
                    continue
                dy, dx = ky - 1, kx - 1
                y0, y1 = max(0, -dy), 32 - max(0, dy)
                x0, x1 = max(0, -dx), 64 - max(0, dx)
                k.stt(ce, ACC, ag[:, y0:y1, x0:x1], xm, xg[:, y0 + dy:y1 + dy, x0 + dx:x1 + dx],
                      cw3[:, cc, ky * 3 + kx:ky * 3 + kx + 1], ACC, ag[:, y0:y1, x0:x1], ALU.mult, ALU.add, extra=[CW])
        for kx in (0, 2):
            dx = kx - 1
            x0, x1 = max(0, -dx), 256 - max(0, dx)
            k.stt(ce, ACC, ACC[:, x0:x1], xm, xm[:, x0 + dx:x1 + dx], cw3[:, cc, 3 + kx:3 + kx + 1],
                  ACC, ACC[:, x0:x1], ALU.mult, ALU.add, extra=[CW])
        k.act(xc, xc.ap, ACC, ACC.ap, AF.Silu, bias=av3[:, 0, cc:cc + 1], extra=[AV])
        if dbg_cc == cc:
            k.debug_out('d_xc', xc, xc.ap, [128, NT], BF16)
            k.debug_out('d_xm', xm, xm.ap, [128, NT])
        for hb in range(2):
            ps = k.bank(0, 7)
            for dch in range(16):
                k.mm(ps, ps.ap, wz, wz3[:, dch, :], HXT, hx3[:, dch, 256 + hb * 512:256 + (hb + 1) * 512],
                     start=(dch == 0), stop=(dch == 15))
            k.act(SZS, SZS[:, hb * 512:(hb + 1) * 512], ps, ps.ap, AF.Silu)
        k.dma('sp', k.SZO, k.SZO.ap[cc], SZS, SZS.ap)

    def stage2(cc):
        xm, xmb, xc = XM[cc % 2], XMB[cc % 2], XC[cc % 2]
        for (i, dst, scr) in ((0, QTS, k.QT), (1, KTS, k.KT)):
            for hb in range(2):
                ps = k.bank(0, 7)
                k.mm(ps, ps.ap, BD, bd4[:, i, cc, :], xc, xc[:, 256 + hb * 512:256 + (hb + 1) * 512])
                k.cp('act', dst, dst[:, hb * 512:(hb + 1) * 512], ps, ps.ap)
            k.dma('sp', scr, scr.ap[cc], dst, dst.ap)
        for (src, i, dst, scr) in ((xc, 1, KS, k.KTOK), (xmb, 2, VS, k.VTOK)):
            for g0 in range(0, 18, 4):
                ps = k.bank(0, 7)
                n = min(4, 18 - g0)
                for j in range(n):
                    tt = g0 + j
                    k.mm(ps, ps[:, j * 128:(j + 1) * 128], src, src[:, tt * 128:(tt + 1) * 128], BD, bd4[:, i, cc, :])
                k.cp('act', dst, dst[:, g0 * 128:(g0 + n) * 128], ps, ps[:, 0:n * 128])
            k.dma('sp', scr, scr.ap[:, :, cc * 128:(cc + 1) * 128].rearrange("t p c -> p t c"), dst, r3(dst.ap, 128))
        GPS = k.bank(0, 7)
        gps3 = r3(GPS.ap[:, 0:288], 16)
        for tt in range(18):
            k.mm(GPS, gps3[:, tt, :], xc, xc[:, tt * 128:(tt + 1) * 128], G, g3[:, cc, 0:16], start=True, stop=False)
            k.mm(GPS, gps3[:, tt, :], xmb, xmb[:, tt * 128:(tt + 1) * 128], G, g3[:, cc, 16:32], start=False, stop=True)
        if cc == 0:
            k.cp('dve', GTM, GTM.ap, GPS, GPS[:, 0:288])
        else:
            k.tt('dve', GTM, GTM.ap, GPS, GPS[:, 0:288], GTM, GTM.ap, ALU.add)
        k.dma('sp', k.XCO, k.XCO.ap[cc], xc, xc[:, 256:256 + NOWN])

    stage1(0)
    for cc in range(ncc):
        if cc + 1 < ncc:
            stage1(cc + 1)
        stage2(cc)
    k.release()


def seq_order(d):
    if d == 0:
        return [(0, False), (1, False)] + [(2 + c, True) for c in range(8)]
    return [(1, False), (0, False)] + [(2 + c, False) for c in range(15, 7, -1)] + [(2 + c, True) for c in range(7, -1, -1)]


def phase_G(k):
    I, CF = k.I, k.CF
    COLS, INTB = k.COLS, k.INTB
    SC = k.dscr('s_inter', [2, 4, 18], F32)
    k.mark()
    GR = k.alloc('gr', NT)
    GK = k.alloc('gk', 4 * NT)
    RW = k.alloc('rw', NT)
    BG = k.alloc('bg', 4)
    k.dma('sp', BG, BG[0:4, :], I['a_bg'], I['a_bg'].ap)
    gtm3 = r3(k.GTM.ap, 16)
    for tt in range(18):
        pbk = k.pb[(tt * 128) // 512]
        c0 = (tt * 128) % 512
        k.mm(pbk, pbk[0:16, c0:c0 + 128], k.GTM, gtm3[:, tt, :], CF, CF[:, C_ID:C_ID + 128])
    for bk in range(5):
        n = 512 if bk < 4 else 256
        k.cp('act', GR, GR[0:16, bk * 512:bk * 512 + n], k.pb[bk], k.pb[bk][0:16, 0:n])
    for kind in range(4):
        k.dma('sp', GK, GK[0:4, kind * NT:(kind + 1) * NT], GR, GR[kind * 4:(kind + 1) * 4, :])
    T = [k.alloc('gt%d' % i, NT) for i in range(8)]
    SM = k.alloc('gsm', 128)
    R4 = lambda b: b[0:4, :]
    V3 = lambda b: r3(b[0:4, :], 128)
    for d in range(2):
        IG, Z, AZ, LF, B0, B1, A, U = T
        k.ts('dve', IG, R4(IG), GK, GK[0:4, (2 * d) * NT:(2 * d + 1) * NT], BG[0:4, 2 * d:2 * d + 1], None, ALU.add, extra=[BG])
        k.ts('dve', Z, R4(Z), GK, GK[0:4, (2 * d + 1) * NT:(2 * d + 2) * NT], BG[0:4, 2 * d + 1:2 * d + 2], None, ALU.add, extra=[BG])
        k.act(AZ, R4(AZ), Z, R4(Z), AF.Abs)
        k.act(AZ, R4(AZ), AZ, R4(AZ), AF.Exp, scale=-1.0)
        k.act(AZ, R4(AZ), AZ, R4(AZ), AF.Ln, bias=1.0)
        k.ts('dve', LF, R4(LF), Z, R4(Z), 0.0, None, ALU.min)
        k.tt('dve', B0, R4(B0), LF, R4(LF), AZ, R4(AZ), ALU.subtract)
        cur, nxt = B0, B1
        sh = 1
        while sh < 128:
            c3, n3 = V3(cur), V3(nxt)
            if d == 0:
                k.cp('dve', nxt, n3[:, :, 0:sh], cur, c3[:, :, 0:sh])
                k.tt('dve', nxt, n3[:, :, sh:128], cur, c3[:, :, sh:128], cur, c3[:, :, 0:128 - sh], ALU.add)
            else:
                k.cp('dve', nxt, n3[:, :, 128 - sh:128], cur, c3[:, :, 128 - sh:128])
                k.tt('dve', nxt, n3[:, :, 0:128 - sh], cur, c3[:, :, 0:128 - sh], cur, c3[:, :, sh:128], ALU.add)
            cur, nxt = nxt, cur
            sh *= 2
        Bc = cur
        Tm = nxt
        k.tt('dve', A, R4(A), IG, R4(IG), Bc, R4(Bc), ALU.subtract)
        AMAX, MC, MM, INTER = SM[0:4, 0:18], SM[0:4, 18:36], SM[0:4, 36:54], SM[0:4, 54:72]
        k.S.op('dve', lambda e, A=A, AMAX=AMAX: e.tensor_reduce(out=AMAX, in_=V3(A), axis=AX.X, op=ALU.max), reads=[A], writes=[SM])
        bend = V3(Bc)[:, :, 127] if d == 0 else V3(Bc)[:, :, 0]
        order = [t for (t, _) in seq_order(d)] if d == 1 else list(range(18))
        k.S.op('dve', lambda e, MM=MM: e.memset(MM, NEG), writes=[SM])
        for i, t in enumerate(order):
            k.tt('dve', SM, MC[:, t:t + 1], SM, MM[:, t:t + 1], SM, AMAX[:, t:t + 1], ALU.max)
            if i + 1 < len(order):
                tn = order[i + 1]
                k.tt('dve', SM, MM[:, tn:tn + 1], Bc, bend[:, t:t + 1], SM, MC[:, t:t + 1], ALU.add)
        k.tt('dve', SM, INTER, SM, MM, SM, MC, ALU.subtract)
        k.act(SM, INTER, SM, INTER, AF.Exp)
        k.dma('sp', SC, SC.ap[d], SM, INTER)
        mcb = MC.unsqueeze(2).broadcast_to([4, 18, 128])
        k.tt('dve', U, V3(U), A, V3(A), SM, mcb, ALU.subtract)
        k.act(U, R4(U), U, R4(U), AF.Exp, bias=k.LN32[0:4, 0:1], extra=[k.LN32])
        k.tt('dve', Tm, V3(Tm), Bc, V3(Bc), SM, mcb, ALU.add)
        k.act(Tm, R4(Tm), Tm, R4(Tm), AF.Exp, scale=-1.0)
        k.dma('sp', RW, RW[(2 * d) * 4:(2 * d) * 4 + 4, :], U, R4(U))
        k.dma('sp', RW, RW[(2 * d + 1) * 4:(2 * d + 1) * 4 + 4, :], Tm, R4(Tm))
    ps = k.pb[5]
    for tt in range(18):
        k.mm(ps, ps[:, tt * 16:(tt + 1) * 16], RW, RW[0:16, tt * 128:(tt + 1) * 128], CF, CF[0:16, C_ID:C_ID + 16])
    k.cp('dve', COLS, COLS.ap, ps, ps[:, 0:288])
    k.dma('sp', INTB, INTB.ap, SC, SC.ap.rearrange("d h t -> (d h t)").partition_broadcast(128))
    k.release()


def phase_scan(k, heads=(0, 1, 2, 3), dbg_hs=None):
    I, CF, CB = k.I, k.CF, k.CB
    COLS, INTB = k.COLS, k.INTB
    cols3 = r3(COLS.ap, 16)
    GT = k.alloc('gt', 32 * NOWN, BF16)
    k.GT = GT
    gt3 = r3(GT.ap, NOWN)
    k.mark()
    AV = k.alloc('avec', 96)
    k.dma('sp', AV, AV.ap, I['a_vec'], I['a_vec'].ap.rearrange("p a b -> p (a b)"))
    av3 = r3(AV.ap, 32)
    CT = [k.alloc('ct%d' % j, 1024) for j in range(8)]
    CTB = [k.alloc('ctb%d' % j, 1024, BF16) for j in range(8)]
    NV = k.alloc('nv', 8)
    NVB = k.alloc('nvb', 8, BF16)
    HS = [k.alloc('hs%d' % c, 1024) for c in range(8)]
    KK = [k.alloc('kk%d' % i, 1024, BF16) for i in range(2)]
    VV = [k.alloc('vv%d' % i, 1024, BF16) for i in range(2)]
    QC = [k.alloc('qc%d' % i, 1024, BF16) for i in range(2)]
    KC = [k.alloc('kc%d' % i, 1024, BF16) for i in range(2)]
    KP = k.alloc('kp', 1024, BF16)
    QTI = k.alloc('qti', 1024, BF16)
    ST = k.alloc('st', 128, BF16)
    DN = k.alloc('dn', 2)
    HN = k.alloc('hn', 8 * 1024, BF16)
    hn3 = r3(HN.ap, 1024)
    STAT = k.alloc('stat', 2 * 6 + 2 + 2)
    SX, TMPF = HS[0], HS[1]
    XCL = [HS[2], HS[3]]
    SZL = [HS[4], HS[5]]
    ONEB = CB[:, C_ONES:C_ONES + 1]
    IDB = CB[:, C_ID:C_ID + 128]
    P_S, P_D = k.pb[0], k.pb[1]
    P_N = Buf(k.psum[:, 1024:2048], 'pn')
    P_C = [Buf(k.psum[:, 2048:3072], 'pc0'), Buf(k.psum[:, 3072:4096], 'pc1')]
    npc = 0
    for h in heads:
        for d in range(2):
            for j in range(8):
                k.S.op('pool', lambda e, t=CT[j]: e.memset(t.ap, 0.0), writes=[CT[j]])
                k.S.op('pool', lambda e, t=CTB[j]: e.memset(t.ap, 0.0), writes=[CTB[j]])
            k.S.op('pool', lambda e: e.memset(NV.ap, 0.0), writes=[NV])
            k.S.op('pool', lambda e: e.memset(NVB.ap, 0.0), writes=[NVB])
            seq = seq_order(d)
            maskT = CB[:, C_U:C_U + 128] if d == 0 else CB[:, C_L:C_L + 128]

            def loads(i):
                tile, wo = seq[i]
                kk, vv = KK[i % 2], VV[i % 2]
                k.dma('sp', kk, kk.ap, k.KTOK, k.KTOK.ap[tile, :, h * 1024:(h + 1) * 1024])
                k.dma('act', vv, vv.ap, k.VTOK, k.VTOK.ap[tile, :, h * 1024:(h + 1) * 1024])
                if wo:
                    c = tile - 2
                    qc, kc = QC[i % 2], KC[i % 2]
                    k.dma('sp', qc, r3(qc.ap, 128), k.QT, k.QT.ap[h * 8:(h + 1) * 8, :, c * 128:(c + 1) * 128].rearrange("j p t -> p j t"))
                    k.dma('act', kc, r3(kc.ap, 128), k.KT, k.KT.ap[h * 8:(h + 1) * 8, :, c * 128:(c + 1) * 128].rearrange("j p t -> p j t"))
            loads(0)
            for i, (tile, wo) in enumerate(seq):
                if i + 1 < len(seq):
                    loads(i + 1)
                kk, vv, qc, kc = KK[i % 2], VV[i % 2], QC[i % 2], KC[i % 2]
                qc3, kc3 = r3(qc.ap, 128), r3(kc.ap, 128)
                ucol = cols3[:, tile, (d * 2) * 4 + h:(d * 2) * 4 + h + 1]
                fcol = cols3[:, tile, (d * 2 + 1) * 4 + h:(d * 2 + 1) * 4 + h + 1]
                icol = INTB[:, d * 72 + h * 18 + tile:d * 72 + h * 18 + tile + 1]
                k.act(KP, KP.ap, kk, kk.ap, AF.Copy, scale=ucol, extra=[COLS])
                if wo:
                    c = tile - 2
                    for j in range(8):
                        k.mm(P_S, P_S[:, 0:128], kc, kc3[:, j, :], qc, qc3[:, j, :], start=(j == 0), stop=(j == 7))
                    k.stt('dve', ST, ST.ap, P_S, P_S[:, 0:128], ucol, CB, maskT, ALU.mult, ALU.mult, extra=[COLS])
                    k.ts('dve', QTI, QTI.ap, qc, qc.ap, icol, None, ALU.mult, extra=[INTB])
                    qti3 = r3(QTI.ap, 128)
                    for half in range(2):
                        k.mm(P_N, P_N[:, half * 512:(half + 1) * 512], ST, ST.ap, vv, vv[:, half * 512:(half + 1) * 512], start=True, stop=False)
                        for j in range(8):
                            k.mm(P_N, P_N[:, half * 512:(half + 1) * 512], QTI, qti3[:, j, :], CTB[j], CTB[j][:, half * 512:(half + 1) * 512],
                                 start=False, stop=(j == 7))
                    k.mm(P_D, P_D[:, 0:1], ST, ST.ap, CB, ONEB, start=True, stop=False)
                    for j in range(8):
                        k.mm(P_D, P_D[:, 0:1], QTI, qti3[:, j, :], NVB, NVB[:, j:j + 1], start=False, stop=(j == 7))
                    k.act(DN, DN[:, 0:1], P_D, P_D[:, 0:1], AF.Abs)
                    k.tt('dve', DN, DN[:, 0:1], DN, DN[:, 0:1], COLS, fcol, ALU.max)
                    k.S.op('dve', lambda e: e.reciprocal(out=DN[:, 1:2], in_=DN[:, 0:1]), reads=[DN], writes=[DN])
                    if d == 0:
                        k.act(HS[c], HS[c].ap, P_N, P_N.ap, AF.Copy, scale=DN[:, 1:2], extra=[DN])
                    else:
                        k.stt('dve', HS[c], HS[c].ap, P_N, P_N.ap, DN[:, 1:2], HS[c], HS[c].ap, ALU.mult, ALU.add, extra=[DN])
                for j in range(8):
                    pc = P_C[npc % 2]
                    npc += 1
                    for half in range(2):
                        k.mm(pc, pc[:, half * 512:(half + 1) * 512], KP, KP[:, j * 128:(j + 1) * 128], vv, vv[:, half * 512:(half + 1) * 512])
                    k.stt('dve', CT[j], CT[j].ap, CT[j], CT[j].ap, icol, pc, pc.ap, ALU.mult, ALU.add, extra=[INTB])
                    k.cp('act', CTB[j], CTB[j].ap, CT[j], CT[j].ap)
                for j in range(8):
                    k.mm(P_D, P_D[:, 8 + j:9 + j], KP, KP[:, j * 128:(j + 1) * 128], CB, ONEB)
                k.stt('dve', NV, NV.ap, NV, NV.ap, icol, P_D, P_D[:, 8:16], ALU.mult, ALU.add, extra=[INTB])
                k.cp('dve', NVB, NVB.ap, NV, NV.ap)
        if dbg_hs == h:
            for c in range(8):
                k.debug_out('d_hs%d' % c, HS[c], HS[c].ap, [128, 1024])
        for c in range(8):
            st6 = r3(STAT[:, 0:12], 6)
            for q in range(2):
                k.S.op('dve', lambda e, c=c, q=q: e.bn_stats(out=st6[:, q, :], in_=HS[c][:, q * 512:(q + 1) * 512]), reads=[HS[c]], writes=[STAT])
            k.S.op('dve', lambda e: e.bn_aggr(out=STAT[:, 12:14], in_=st6), reads=[STAT], writes=[STAT])
            k.act(STAT, STAT[:, 15:16], STAT, STAT[:, 13:14], AF.Sqrt, bias=k.EPS[:, 0:1], extra=[k.EPS])
            k.S.op('dve', lambda e: e.reciprocal(out=STAT[:, 14:15], in_=STAT[:, 15:16]), reads=[STAT], writes=[STAT])
            k.ts('dve', HN, hn3[:, c, :], HS[c], HS[c].ap, STAT[:, 12:13], STAT[:, 14:15], ALU.subtract, ALU.mult, extra=[STAT])
        for j in range(8):
            cc = h * 8 + j
            xcl, szl = XCL[j % 2], SZL[j % 2]
            xcl_ap = xcl.ap[:, 0:512].bitcast(BF16)
            szl_ap = szl.ap[:, 0:512].bitcast(BF16)
            k.dma('sp', xcl, xcl_ap, k.XCO, k.XCO.ap[cc])
            k.dma('act', szl, szl_ap, k.SZO, k.SZO.ap[cc])
            for c in range(8):
                k.mm(P_N, P_N[:, c * 128:(c + 1) * 128], HN, hn3[:, c, j * 128:(j + 1) * 128], CB, IDB)
            k.act(SX, SX.ap, xcl, xcl_ap, AF.Copy, scale=av3[:, 2, cc:cc + 1], extra=[AV])
            k.stt('dve', TMPF, TMPF.ap, P_N, P_N.ap, av3[:, 1, cc:cc + 1], SX, SX.ap, ALU.mult, ALU.add, extra=[AV])
            k.tt('pool', GT, gt3[:, cc, :], TMPF, TMPF.ap, szl, szl_ap, ALU.mult)
    k.release()


def lnp_col(k, layer, which, dch):
    v = k.LNP.ap.rearrange("p (a b c) -> p a b c", a=2, b=4)
    return v[:, layer, which, dch:dch + 1]


def modv(k, layer, part, dch, r=0):
    return r3(k.MODV.ap, 2)[:, mod_col(layer, part, dch), r:r + 1]


def mod1(k, layer, part, dch, r=0):
    return r3(k.MOD1.ap, 2)[:, mod_col(layer, part, dch), r:r + 1]


def build_rs0(k):
    I, CF = k.I, k.CF
    rs3 = r3(k.RS.ap, NOWN)
    k.mark()
    XT = [k.alloc('xt%d' % i, D) for i in range(2)]
    for tt in range(8):
        xt = XT[tt % 2]
        k.dma('sp', xt, xt.ap, I['xl'], I['xl'].ap[tt * 128:(tt + 1) * 128, :])
        for g in range(4):
            ps = k.bank()
            for i in range(4):
                dch = g * 4 + i
                k.mm(ps, ps[:, i * 128:(i + 1) * 128], xt, xt[:, dch * 128:(dch + 1) * 128], CF, CF[:, C_ID:C_ID + 128])
            k.act(k.RS, rs3[:, g * 4:(g + 1) * 4, tt * 128:(tt + 1) * 128], ps, r3(ps.ap, 128), AF.Copy, scale=ALPHA)
    k.release()


def phase_wout(k, GT, wname, layer):
    I = k.I
    gt3 = r3(GT.ap, NOWN)
    rs3 = r3(k.RS.ap, NOWN)
    k.mark()
    WO = [k.alloc('wo%d' % i, 32 * 256, BF16) for i in range(2)]
    w = I[wname]
    for p in range(8):
        wo = WO[p % 2]
        wo3 = r3(wo.ap, 256)
        k.dma('pool', wo, wo3, w, w.ap[:, p * 256:(p + 1) * 256].rearrange("(c p) d -> p c d", p=128))
        for dl in range(2):
            dch = p * 2 + dl
            for half in range(2):
                ps = k.bank()
                for cc in range(32):
                    k.mm(ps, ps.ap, wo, wo3[:, cc, dl * 128:(dl + 1) * 128], GT, gt3[:, cc, half * 512:(half + 1) * 512],
                         start=(cc == 0), stop=(cc == 31))
                k.stt('dve', k.RS, rs3[:, dch, half * 512:(half + 1) * 512], ps, ps.ap, modv(k, layer, 2, dch),
                      k.RS, rs3[:, dch, half * 512:(half + 1) * 512], ALU.mult, ALU.add, extra=[k.MODV])
    k.release()


def ln_fm(k, layer, which):
    CF = k.CF
    rs3 = r3(k.RS.ap, NOWN)
    k.mark()
    SQ = [k.alloc('sq%d' % i, 512) for i in range(2)]
    MEAN = k.alloc('mean', 512)
    RSTD = k.alloc('rstd', 512)
    M2 = k.alloc('m2', 512)
    T = [k.alloc('lt%d' % i, 512) for i in range(2)]
    ONES = CF[:, C_ONES:C_ONES + 128]
    for half in range(2):
        sl = slice(half * 512, (half + 1) * 512)
        ps_s, ps_q = k.bank(), k.bank()
        for dch in range(16):
            k.mm(ps_s, ps_s.ap, CF, ONES, k.RS, rs3[:, dch, sl], start=(dch == 0), stop=(dch == 15))
        for dch in range(16):
            sq = SQ[dch % 2]
            k.act(sq, sq.ap, k.RS, rs3[:, dch, sl], AF.Square)
            k.mm(ps_q, ps_q.ap, CF, ONES, sq, sq.ap, start=(dch == 0), stop=(dch == 15))
        k.act(MEAN, MEAN.ap, ps_s, ps_s.ap, AF.Copy, scale=1.0 / D)
        k.tt('pool', M2, M2.ap, MEAN, MEAN.ap, MEAN, MEAN.ap, ALU.mult)
        k.stt('dve', RSTD, RSTD.ap, ps_q, ps_q.ap, 1.0 / D, M2, M2.ap, ALU.mult, ALU.subtract)
        k.act(RSTD, RSTD.ap, RSTD, RSTD.ap, AF.Sqrt, bias=k.EPS[:, 0:1], extra=[k.EPS])
        k.S.op('dve', lambda e: e.reciprocal(out=RSTD.ap, in_=RSTD.ap), reads=[RSTD], writes=[RSTD])
        for dch in range(16):
            t = T[dch % 2]
            k.tt('dve', t, t.ap, k.RS, rs3[:, dch, sl], MEAN, MEAN.ap, ALU.subtract)
            k.tt('pool', t, t.ap, t, t.ap, RSTD, RSTD.ap, ALU.mult)
            k.act(k.RS, rs3[:, dch, sl], t, t.ap, AF.Identity, scale=lnp_col(k, layer, which, dch),
                  bias=lnp_col(k, layer, which + 1, dch), extra=[k.LNP])
    k.release()


def phase_moe(k, layer, nexp=NEXP):
    I, CF, CB = k.I, k.CF, k.CB
    rs3 = r3(k.RS.ap, NOWN)
    IDB = CB[:, C_ID:C_ID + 128]
    k.mark()
    TOK = k.alloc('tok', 8 * D, BF16)
    tok3 = r3(TOK.ap, D)
    MASK = k.alloc('mask', 256)
    RANK = k.alloc('rank', 256)
    GHL = k.alloc('ghl', 512, BF16)
    MASKB = k.alloc('maskb', 256, BF16)
    mask3, rank3, maskb3 = [r3(b.ap, 32) for b in (MASK, RANK, MASKB)]
    ghl4 = GHL.ap.rearrange("p (t e two) -> p t e two", t=8, two=2)
    B1E = [k.alloc('b1e%d' % i, 32) for i in range(2)]
    k.mark()
    LG = k.alloc('lg', 256)
    GATE = k.alloc('gate', 256)
    lg3, gate3 = r3(LG.ap, 32), r3(GATE.ap, 32)
    TKF = k.alloc('tkf', 16 * NOWN)
    tkf3 = r3(TKF.ap, NOWN)
    TKB = [k.alloc('tkb%d' % i, NOWN, BF16) for i in range(2)]
    RWF = k.alloc('rwf', 16 * 32)
    RB = k.alloc('rb', 32)
    SMT = k.alloc('smt', 64)
    k.dma('sp', RWF, r3(RWF.ap, 32), I['r_w'], I['r_w'].ap[layer].rearrange("(j p) e -> p j e", p=128))
    k.dma('sp', RB, RB.ap, I['r_b'], I['r_b'].ap[layer, :].partition_broadcast(128))
    rwf3 = r3(RWF.ap, 32)
    for dch in range(16):
        k.act(TKF, tkf3[:, dch, :], k.RS, rs3[:, dch, :], AF.Identity, scale=mod1(k, layer, 4, dch), bias=modv(k, layer, 3, dch),
              extra=[k.MOD1, k.MODV])
        tkb = TKB[dch % 2]
        k.cp('pool', tkb, tkb.ap, TKF, tkf3[:, dch, :])
        for tg in range(2):
            ps = k.bank()
            for j in range(4):
                t = tg * 4 + j
                k.mm(ps, ps[:, j * 128:(j + 1) * 128], tkb, tkb[:, t * 128:(t + 1) * 128], CB, IDB)
            k.cp('dve', TOK, tok3[:, tg * 4:(tg + 1) * 4, dch * 128:(dch + 1) * 128], ps, r3(ps.ap, 128))
    for dch in range(16):
        k.S.op('pool', lambda e, dch=dch: e.tensor_scalar(out=rs3[:, dch, :], in0=rs3[:, dch, :], scalar1=ALPHA, scalar2=None, op0=ALU.mult),
               reads=[k.RS], writes=[k.RS])
    for t in range(8):
        ps = k.bank()
        for dch in range(16):
            k.mm(ps, ps[:, 0:32], TKF, tkf3[:, dch, t * 128:(t + 1) * 128], RWF, rwf3[:, dch, :], start=(dch == 0), stop=(dch == 15))
        k.tt('dve', LG, lg3[:, t, :], ps, ps[:, 0:32], RB, RB.ap, ALU.add)
    for t in range(8):
        MX = SMT[:, 0:8]
        k.S.op('dve', lambda e, t=t: e.max(out=SMT[:, 0:8], in_=lg3[:, t, :]), reads=[LG], writes=[SMT])
        k.ts('dve', MASK, mask3[:, t, :], LG, lg3[:, t, :], SMT[:, 3:4], None, ALU.is_ge, extra=[SMT])
        k.ts('dve', SMT, SMT[:, 8:9], SMT, SMT[:, 0:1], -1.0, None, ALU.mult)
        k.act(GATE, gate3[:, t, :], LG, lg3[:, t, :], AF.Exp, bias=SMT[:, 8:9], extra=[SMT])
        k.tt('dve', GATE, gate3[:, t, :], GATE, gate3[:, t, :], MASK, mask3[:, t, :], ALU.mult)
        k.S.op('dve', lambda e, t=t: e.reduce_sum(out=SMT[:, 9:10], in_=gate3[:, t, :], axis=AX.X), reads=[GATE], writes=[SMT])
        k.S.op('dve', lambda e: e.reciprocal(out=SMT[:, 10:11], in_=SMT[:, 9:10]), reads=[SMT], writes=[SMT])
        k.ts('dve', GATE, gate3[:, t, :], GATE, gate3[:, t, :], SMT[:, 10:11], None, ALU.mult, extra=[SMT])
    k.cp('dve', GHL, ghl4[:, :, :, 0], GATE, gate3)
    k.cp('dve', LG, lg3, GHL, ghl4[:, :, :, 0])
    k.tt('dve', LG, LG.ap, GATE, GATE.ap, LG, LG.ap, ALU.subtract)
    k.cp('dve', GHL, ghl4[:, :, :, 1], LG, lg3)
    k.cp('dve', MASKB, MASKB.ap, MASK, MASK.ap)
    CNT = k.alloc('cnt', 32)
    k.S.op('dve', lambda e: e.tensor_reduce(out=CNT.ap, in_=mask3.rearrange("p t e -> p e t"), axis=AX.X, op=ALU.add), reads=[MASK], writes=[CNT])
    pcn = k.bank()
    k.mm(pcn, pcn[:, 0:32], CF, CF[:, C_ONES:C_ONES + 128], CNT, CNT.ap)
    k.cp('dve', CNT, CNT.ap, pcn, pcn[:, 0:32])
    co = k.dout('cnt%d' % layer, [1, 32])
    k.dma('sp', co, co.ap, CNT, CNT[0:1, :])
    for t in range(8):
        ps = k.bank()
        k.mm(ps, ps[:, 0:32], CB, CB[:, C_TRI:C_TRI + 128], MASKB, maskb3[:, t, :], start=True, stop=(t == 0))
        for i in range(t):
            k.mm(ps, ps[:, 0:32], CB, CB[:, C_ONES:C_ONES + 128], MASKB, maskb3[:, i, :], start=False, stop=(i == t - 1))
        k.cp('dve', RANK, rank3[:, t, :], ps, ps[:, 0:32])
    k.release()
    PM = k.alloc('pm', 8 * CAP, BF16)
    pm3 = r3(PM.ap, CAP)
    PT = k.alloc('pt', NSB * NOWN, BF16)
    pt3 = r3(PT.ap, NOWN)
    GS = k.alloc('gs', NSB)
    GS4 = k.alloc('gs4', 2 * NSB)
    XE = k.alloc('xe', 16 * CAP, BF16)
    xe3 = r3(XE.ap, CAP)
    ACTT = k.alloc('actt', 16 * CAP, BF16)
    at3 = r3(ACTT.ap, CAP)
    YE = [k.alloc('ye%d' % i, NSB * 256, BF16) for i in range(2)]
    w1, w2, b2 = I['e_w1'], I['e_w2'], I['e_b2']
    NB = 6
    WR = [k.alloc('wr%d' % i, 16 * 256, BF16) for i in range(NB)]
    B2B1 = k.alloc('b2b', D, BF16)
    blocks = []
    for e_ in range(nexp):
        for fb_ in range(8):
            blocks.append(('g', e_, fb_))
            blocks.append(('l', e_, fb_))
        blocks.append(('b2', e_, 0))
        for db_ in range(8):
            blocks.append(('w2', e_, db_))
    wst = {'issued': 0, 'ring': 0}
    slot_of = {}

    def issue_upto(n):
        while wst['issued'] < min(n, len(blocks)):
            kind, e_, idx = blocks[wst['issued']]
            if kind == 'b2':
                k.dma('pool', B2B1, B2B1[0:1, :], b2, b2.ap[layer, e_:e_ + 1, :])
            else:
                buf = WR[wst['ring'] % NB]
                wst['ring'] += 1
                slot_of[(kind, e_, idx)] = buf
                if kind == 'g':
                    src = w1.ap[layer, e_, :, idx * 256:(idx + 1) * 256]
                elif kind == 'l':
                    src = w1.ap[layer, e_, :, D + idx * 256:D + (idx + 1) * 256]
                else:
                    src = w2.ap[layer, e_, :, idx * 256:(idx + 1) * 256]
                k.dma('pool', buf, r3(buf.ap, 256), w1 if kind != 'w2' else w2, src.rearrange("(j p) c -> p j c", p=128))
            wst['issued'] += 1

    TG = k.alloc('tg', CAP)
    TL = k.alloc('tl', CAP)
    TS_ = k.alloc('tsg', CAP)
    IOTA = CF[:, C_IOTA:C_IOTA + CAP]
    nyc = [0]
    issue_upto(NB)
    def f_pm(e):
        for t in range(8):
            k.ts('dve', PM, pm3[:, t, :], CF, IOTA, rank3[:, t, e:e + 1], mask3[:, t, e:e + 1], ALU.is_equal, ALU.mult, extra=[RANK, MASK])

    def f_gather(e):
        for dch in range(16):
            ps = k.bank()
            for t in range(8):
                k.mm(ps, ps[:, 0:CAP], TOK, tok3[:, t, dch * 128:(dch + 1) * 128], PM, pm3[:, t, :], start=(t == 0), stop=(t == 7))
            k.cp('act' if dch % 2 else 'dve', XE, xe3[:, dch, :], ps, ps[:, 0:CAP])

    def f_sel(e):
        b1e = B1E[e % 2]
        k.dma('sp', b1e, b1e.ap, I['e_b1'], I['e_b1'].ap[:, layer, e, :])
        for sb in range(NSB):
            for tg in range(2):
                ps = k.bank()
                for j in range(4):
                    t = tg * 4 + j
                    k.mm(ps, ps[:, j * 128:(j + 1) * 128], PM, pm3[:, t, sb * 128:(sb + 1) * 128], CB, IDB)
                k.cp('act', PT, pt3[:, sb, tg * 512:(tg + 1) * 512], ps, ps.ap)
        ps = k.bank()
        for sb in range(NSB):
            for t in range(8):
                k.mm(ps, ps[:, sb * 2:sb * 2 + 2], PM, pm3[:, t, sb * 128:(sb + 1) * 128], GHL, ghl4[:, t, e, :], start=(t == 0), stop=(t == 7))
        k.cp('act', GS4, GS4.ap, ps, ps[:, 0:2 * NSB])
        gs43 = r3(GS4.ap, 2)
        k.tt('dve', GS, GS.ap, GS4, gs43[:, :, 0], GS4, gs43[:, :, 1], ALU.add)

        return b1e

    def f_w1(e, b1e):
        for fb in range(8):
            issue_upto(e * 25 + fb * 2 + NB)
            wg_, wl_ = slot_of[('g', e, fb)], slot_of[('l', e, fb)]
            wg3_, wl3_ = r3(wg_.ap, 256), r3(wl_.ap, 256)
            for fl in range(2):
                fch = fb * 2 + fl
                psg_, psl_ = k.bank(), k.bank()
                for dch in range(16):
                    k.mm(psg_, psg_[:, 0:CAP], wg_, wg3_[:, dch, fl * 128:(fl + 1) * 128], XE, xe3[:, dch, :], start=(dch == 0), stop=(dch == 15))
                for dch in range(16):
                    k.mm(psl_, psl_[:, 0:CAP], wl_, wl3_[:, dch, fl * 128:(fl + 1) * 128], XE, xe3[:, dch, :], start=(dch == 0), stop=(dch == 15))
                k.ts('dve', TG, TG.ap, psg_, psg_[:, 0:CAP], b1e[:, fch:fch + 1], 7.0, ALU.add, ALU.min, extra=[b1e])
                k.ts('dve', TL, TL.ap, psl_, psl_[:, 0:CAP], b1e[:, 16 + fch:17 + fch], 7.0, ALU.add, ALU.min, extra=[b1e])
                k.ts('dve', TL, TL.ap, TL, TL.ap, -7.0, 1.0, ALU.max, ALU.add)
                k.act(TS_, TS_.ap, TG, TG.ap, AF.Sigmoid, scale=1.702)
                k.tt('dve', TS_, TS_.ap, TS_, TS_.ap, TG, TG.ap, ALU.mult)
                k.tt('dve', ACTT, at3[:, fch, :], TS_, TS_.ap, TL, TL.ap, ALU.mult)

    def f_w2(e):
        for db in range(8):
            issue_upto(e * 25 + 17 + db + NB)
            w2_ = slot_of[('w2', e, db)]
            w23 = r3(w2_.ap, 256)
            ye = YE[nyc[0] % 2]
            nyc[0] += 1
            ye3 = r3(ye.ap, 256)
            for sb in range(NSB):
                if sb % 2 == 0:
                    ps = k.bank()
                c0 = (sb % 2) * 256
                for fch in range(16):
                    k.mm(ps, ps[:, c0:c0 + 256], ACTT, at3[:, fch, sb * 128:(sb + 1) * 128], w2_, w23[:, fch, :], start=(fch == 0), stop=False)
                k.mm(ps, ps[:, c0:c0 + 256], CB, CB[0:1, C_ONES:C_ONES + 128], B2B1, B2B1[0:1, db * 256:(db + 1) * 256], start=False, stop=True)
                k.act(ye, ye3[:, sb, :], ps, ps[:, c0:c0 + 256], AF.Copy, scale=GS[:, sb:sb + 1], extra=[GS])
            for dl in range(2):
                dch = db * 2 + dl
                for half in range(2):
                    ps = k.bank()
                    for sb in range(NSB):
                        k.mm(ps, ps.ap, ye, ye3[:, sb, dl * 128:(dl + 1) * 128], PT, pt3[:, sb, half * 512:(half + 1) * 512], start=(sb == 0), stop=(sb == NSB - 1))
                    k.stt('dve', k.RS, rs3[:, dch, half * 512:(half + 1) * 512], ps, ps.ap, modv(k, layer, 5, dch),
                          k.RS, rs3[:, dch, half * 512:(half + 1) * 512], ALU.mult, ALU.add, extra=[k.MODV])

    f_pm(0)
    f_gather(0)
    for e in range(nexp):
        b1e = f_sel(e)
        if e + 1 < nexp:
            f_pm(e + 1)
        f_w1(e, b1e)
        if e + 1 < nexp:
            f_gather(e + 1)
        f_w2(e)
    k.release()


def phase_B(k):
    I, CF, CB = k.I, k.CF, k.CB
    rs3 = r3(k.RS.ap, NOWN)
    G1 = k.alloc('g1', 32 * NOWN, BF16)
    k.G1 = G1
    g13 = r3(G1.ap, NOWN)
    VGS = k.dscr('s_vg', [8, 128, E_IN])
    k.mark()
    HX1 = k.alloc('hx1', 16 * NOWN, BF16)
    hx3 = r3(HX1.ap, NOWN)
    for dch in range(16):
        k.act(HX1, hx3[:, dch, :], k.RS, rs3[:, dch, :], AF.Identity, scale=mod1(k, 1, 1, dch), bias=modv(k, 1, 0, dch), extra=[k.MOD1, k.MODV])
    for dch in range(16):
        k.S.op('pool', lambda e, dch=dch: e.tensor_scalar(out=rs3[:, dch, :], in0=rs3[:, dch, :], scalar1=ALPHA, scalar2=None, op0=ALU.mult),
               reads=[k.RS], writes=[k.RS])
    MV = k.alloc('mv', 8 * 2)
    RSTDV = k.alloc('rstdv', 8)
    mv3 = r3(MV.ap, 2)
    w = I['b_w_in']
    k.mark()
    STATS = k.alloc('stats', 8 * 16 * 6)
    st4 = STATS.ap.rearrange("p (t c s) -> p t c s", t=8, c=16)
    WV = [k.alloc('wv%d' % i, 16 * 256, BF16) for i in range(2)]
    VGT = [k.alloc('vgt%d' % i, 256) for i in range(2)]
    VGB = [k.alloc('vgb%d' % i, 256, BF16) for i in range(2)]
    n = 0
    def load_wv(cb_):
        wv_ = WV[cb_ % 2]
        k.dma('pool', wv_, r3(wv_.ap, 256), w, w.ap[:, E_IN + cb_ * 256:E_IN + (cb_ + 1) * 256].rearrange("(j p) c -> p j c", p=128))
    load_wv(0)
    for cb in range(16):
        wv = WV[cb % 2]
        wv3 = r3(wv.ap, 256)
        if cb + 1 < 16:
            load_wv(cb + 1)
        for t in range(8):
            ps = k.bank()
            for dch in range(16):
                k.mm(ps, ps[:, 0:256], HX1, hx3[:, dch, t * 128:(t + 1) * 128], wv, wv3[:, dch, :], start=(dch == 0), stop=(dch == 15))
            vgt, vgb = VGT[n % 2], VGB[n % 2]
            n += 1
            k.act(vgt, vgt.ap, ps, ps[:, 0:256], AF.Gelu)
            k.S.op('dve', lambda e, t=t, cb=cb, vgt=vgt: e.bn_stats(out=st4[:, t, cb, :], in_=vgt.ap), reads=[vgt], writes=[STATS])
            k.cp('pool', vgb, vgb.ap, vgt, vgt.ap)
            k.dma('sp', VGS, VGS.ap[t, :, cb * 256:(cb + 1) * 256], vgb, vgb.ap)
    for t in range(8):
        k.S.op('dve', lambda e, t=t: e.bn_aggr(out=mv3[:, t, :], in_=st4[:, t, :, :]), reads=[STATS], writes=[MV])
    k.act(RSTDV, RSTDV.ap, MV, mv3[:, :, 1], AF.Sqrt, bias=k.EPS[:, 0:1], extra=[k.EPS])
    k.S.op('dve', lambda e: e.reciprocal(out=RSTDV.ap, in_=RSTDV.ap), reads=[RSTDV], writes=[RSTDV])
    k.release()
    WSB = k.alloc('wsb', 8 * 128, BF16)
    BSB = k.alloc('bsb', 8 * 128)
    k.mark()
    WSf = k.alloc('wsf', 8 * 128)
    k.dma('sp', WSf, WSf.ap, I['b_wsT'], I['b_wsT'].ap.rearrange("p g t -> p (g t)"))
    k.cp('dve', WSB, WSB.ap, WSf, WSf.ap)
    k.release()
    k.dma('sp', BSB, BSB.ap, I['b_bs'], I['b_bs'].ap.rearrange("g t -> (g t)").partition_broadcast(128))
    wsb3, bsb3 = r3(WSB.ap, 128), r3(BSB.ap, 128)
    VGG = [k.alloc('vgg', 8 * 512, BF16)] * 2
    LNG = [k.alloc('lng', 512)] * 2
    LNB = [k.alloc('lnb', 512)] * 2
    TN = [k.alloc('tn%d' % i, 512) for i in range(2)]
    WU = [k.alloc('wu%d' % i, 16 * 128, BF16) for i in range(2)]
    UT = [k.alloc('ut', NOWN, BF16)] * 2
    TM = k.alloc('tm', NOWN)
    nn = 0
    for g in range(8):
        vgg = VGG[g % 2]
        vgg3 = r3(vgg.ap, 512)
        lng, lnb = LNG[g % 2], LNB[g % 2]
        k.dma('sp', vgg, vgg3, VGS, VGS.ap[:, :, g * 512:(g + 1) * 512].rearrange("c p n -> p c n"))
        k.dma('sp', lng, lng.ap, I['b_lng'], I['b_lng'].ap[0, g * 512:(g + 1) * 512].partition_broadcast(128))
        k.dma('sp', lnb, lnb.ap, I['b_lnb'], I['b_lnb'].ap[0, g * 512:(g + 1) * 512].partition_broadcast(128))
        for c in range(8):
            tn = TN[c % 2]
            k.ts('dve', tn, tn.ap, vgg, vgg3[:, c, :], mv3[:, c, 0:1], RSTDV[:, c:c + 1], ALU.subtract, ALU.mult, extra=[MV, RSTDV])
            k.tt('pool', tn, tn.ap, tn, tn.ap, lng, lng.ap, ALU.mult)
            k.tt('pool', vgg, vgg3[:, c, :], tn, tn.ap, lnb, lnb.ap, ALU.add)
        for cl in range(4):
            cc = g * 4 + cl
            wu = WU[cc % 2]
            wu3 = r3(wu.ap, 128)
            if cc == 0:
                k.dma('pool', wu, wu3, w, w.ap[:, 0:128].rearrange("(j p) c -> p j c", p=128))
            if cc + 1 < 32:
                wun = WU[(cc + 1) % 2]
                k.dma('pool', wun, r3(wun.ap, 128), w, w.ap[:, (cc + 1) * 128:(cc + 2) * 128].rearrange("(j p) c -> p j c", p=128))
            ut = UT[cc % 2]
            for half in range(2):
                ps = k.bank()
                for dch in range(16):
                    k.mm(ps, ps.ap, wu, wu3[:, dch, :], HX1, hx3[:, dch, half * 512:(half + 1) * 512], start=(dch == 0), stop=(dch == 15))
                k.act(ut, ut[:, half * 512:(half + 1) * 512], ps, ps.ap, AF.Gelu)
            for half in range(2):
                ps = k.bank()
                for j in range(4):
                    c = half * 4 + j
                    k.mm(ps, ps[:, j * 128:(j + 1) * 128], vgg, vgg3[:, c, cl * 128:(cl + 1) * 128], WSB, wsb3[:, g, :])
                k.tt('dve', TM, r3(TM[:, half * 512:(half + 1) * 512], 128), ps, r3(ps.ap, 128), BSB,
                     bsb3[:, g, :].unsqueeze(1).broadcast_to([128, 4, 128]), ALU.add)
            k.tt('pool', G1, g13[:, cc, :], TM, TM.ap, ut, ut.ap, ALU.mult)
    k.release()


def phase_out(k, OUT):
    CF = k.CF
    rs3 = r3(k.RS.ap, NOWN)
    k.mark()
    OT = [k.alloc('ot%d' % i, D) for i in range(2)]
    for t in range(8):
        ot = OT[t % 2]
        for g in range(4):
            ps = k.bank()
            for i in range(4):
                dch = g * 4 + i
                k.mm(ps, ps[:, i * 128:(i + 1) * 128], k.RS, rs3[:, dch, t * 128:(t + 1) * 128], CF, CF[:, C_ID:C_ID + 128])
            k.cp('act' if g % 2 else 'dve', ot, ot[:, g * 512:(g + 1) * 512], ps, ps.ap)
        k.dma('sp', OUT, OUT.ap[t * 128:(t + 1) * 128, :], ot, ot.ap)
    k.release()


def kernel(**inputs):
    if 'nc' not in _CACHE:
        _CACHE['nc'] = build_program()
    nc = _CACHE['nc']
    names = _CACHE['names']
    sh = prep_shared(inputs)
    in_maps = []
    for core in range(8):
        pc = prep_core(inputs, core, sh)
        allin = {**sh, **pc}
        in_maps.append({n: allin[n] for n in names})
    res = run_bass_kernel_spmd(nc, in_maps, core_ids=list(range(8)))
    out = np.zeros((4, 2048, D), np.float32)
    for core in range(8):
        b, half = core // 2, core % 2
        yl = np.asarray(res.results[core]['yl'], np.float32)
        try:
            print('[moe-load] core', core, 'max tokens/expert L0', float(np.max(res.results[core]['cnt0'])),
                  'L1', float(np.max(res.results[core]['cnt1'])), 'cap', CAP, flush=True)
        except Exception:
            pass
        if half == 0:
            out[b, 0:NOWN] = yl
        else:
            out[b, NOWN:2048] = yl[::-1]
    return out
```
